# Optimizing a Trainium2 kernel written in Bass

```python
import math
import jax, jax.numpy as jnp
from jax import lax
import numpy as np

D_MODEL = 4096
BATCH = 4
SEQ = 2048
DEPTH = 2

N_A_LAYERS = DEPTH // 2
N_B_LAYERS = DEPTH - N_A_LAYERS
N_DENSE_LAYERS = (DEPTH + 1) // 2
N_MOE_LAYERS = DEPTH // 2

HEAD_DIM = 128
N_HEADS = D_MODEL // HEAD_DIM
ATTN_WIDTH = N_HEADS * HEAD_DIM
ROPE_THETA = 10000.0
DILATED_BRANCHES = ((128, 1), (512, 4), (2048, 16))
ATTN_BLOCK = 128

POOL_WINDOWS = (2, 4, 8, 16)
POOL_GROUPS = len(POOL_WINDOWS)
POOL_GROUP_DIM = D_MODEL // POOL_GROUPS

D_FF = 11008 * D_MODEL // 4096
N_EXPERTS = 8
TOP_K = 2
D_FF_EXPERT = 7 * D_MODEL // 8
PLE_DIM = 256
RMS_EPS = 1e-6

kernel_name = "yoco_pool_dilated_moe_hybrid"

F32 = jnp.float32


def rms_norm(x, g):
    xf = x.astype(F32)
    y = xf * lax.rsqrt(jnp.mean(xf * xf, axis=-1, keepdims=True) + RMS_EPS)
    return (y * g.astype(F32)).astype(x.dtype)


def rope(x):
    s = x.shape[1]
    half = HEAD_DIM // 2
    inv_freq = jnp.exp(-math.log(ROPE_THETA) * jnp.arange(half, dtype=F32) / half)
    ang = jnp.arange(s, dtype=F32)[:, None] * inv_freq[None, :]
    cos = jnp.cos(ang)[None, :, None, :]
    sin = jnp.sin(ang)[None, :, None, :]
    xf = x.astype(F32)
    x1, x2 = xf[..., :half], xf[..., half:]
    return jnp.concatenate([x1 * cos - x2 * sin, x2 * cos + x1 * sin], axis=-1).astype(x.dtype)


def multiscale_pool(xn, w_groups, scale):
    b, s, d = xn.shape
    xf = xn.astype(F32)
    cs = jnp.concatenate([jnp.zeros((b, 1, d), F32), jnp.cumsum(xf, axis=1)], axis=1)
    t = jnp.arange(s)
    outs = []
    for g, w in enumerate(POOL_WINDOWS):
        lo_c, hi_c = g * POOL_GROUP_DIM, (g + 1) * POOL_GROUP_DIM
        csg = cs[:, :, lo_c:hi_c]
        lo = jnp.maximum(t + 1 - w, 0)
        win_sum = csg[:, 1:, :] - csg[:, lo, :]
        cnt = jnp.minimum(t + 1, w).astype(F32)[None, :, None]
        outs.append(win_sum / cnt - xf[:, :, lo_c:hi_c])
    pooled = jnp.stack(outs, axis=2).astype(xn.dtype)
    mixed = jnp.einsum('bsgc,gce->bsge', pooled, w_groups).reshape(b, s, d)
    return mixed * scale


def swiglu(x, w_gate_up, w_down):
    g, u = jnp.split(x @ w_gate_up, 2, axis=-1)
    return (jax.nn.silu(g) * u) @ w_down


def moe_swiglu(xn, router, w_gate_up, w_down):
    b, s, d = xn.shape
    xt = xn.reshape(-1, d)
    logits = (xt @ router).astype(F32)
    top_logit, top_idx = lax.top_k(logits, TOP_K)
    gates = jax.nn.softmax(top_logit, axis=-1)
    combine = jnp.sum(jax.nn.one_hot(top_idx, N_EXPERTS, dtype=F32) * gates[..., None], axis=1)
    out = jnp.zeros(xt.shape, F32)
    for e in range(N_EXPERTS):
        out = out + combine[:, e:e + 1] * swiglu(xt, w_gate_up[e], w_down[e]).astype(F32)
    return out.astype(xn.dtype).reshape(b, s, d)


def per_layer_embedding(h, p_i, w_ple, ple_norm, w_ple_gate):
    gate = jax.nn.sigmoid((rms_norm(h, ple_norm) @ w_ple_gate).astype(F32))
    return h + ((p_i @ w_ple).astype(F32) * gate).astype(h.dtype)


def shared_kv(h, kv_norm, w_kv, k_norm):
    b, s, _ = h.shape
    kv = rms_norm(h, kv_norm) @ w_kv
    k, v = jnp.split(kv, 2, axis=-1)
    k = rope(rms_norm(k.reshape(b, s, N_HEADS, HEAD_DIM), k_norm)).astype(F32)
    v = v.reshape(b, s, N_HEADS, HEAD_DIM).astype(F32)
    return k, v


def dilated_branch(q, k, v, window, dilation):
    b, s, h, hd = q.shape
    L = s // dilation
    n_keys = window // dilation
    blk = ATTN_BLOCK
    nb = -(-L // blk)
    Lp = nb * blk
    n = b * dilation

    def to_strided(t):
        t = t.reshape(b, L, dilation, h, hd).transpose(0, 2, 1, 3, 4).reshape(n, L, h, hd)
        return jnp.pad(t, ((0, 0), (0, Lp - L), (0, 0), (0, 0)))

    def band(t):
        tp = jnp.pad(t, ((0, 0), (blk, 0), (0, 0), (0, 0))).reshape(n, nb + 1, blk, h, hd)
        return jnp.concatenate([tp[:, :-1], tp[:, 1:]], axis=2)

    qb = to_strided(q).reshape(n, nb, blk, h, hd)
    kb = band(to_strided(k))
    vb = band(to_strided(v))

    qi = jnp.arange(blk)[:, None]
    kj = jnp.arange(2 * blk)[None, :]
    dist = qi + blk - kj
    kpos = (jnp.arange(nb)[:, None, None] - 1) * blk + kj[None]
    mask = (dist >= 0)[None] & (dist <= n_keys)[None] & (kpos >= 0)

    scores = jnp.einsum('nbqhd,nbkhd->nbhqk', qb, kb) / math.sqrt(hd)
    scores = jnp.where(mask[None, :, None], scores, -jnp.inf)
    m = jnp.max(scores, axis=-1, keepdims=True)
    e = jnp.exp(scores - m)
    den = jnp.sum(e, axis=-1, keepdims=True)
    o = jnp.einsum('nbhqk,nbkhd->nbqhd', e / den, vb)
    lse = (m + jnp.log(den))[..., 0].transpose(0, 1, 3, 2)

    def from_strided(t):
        rest = t.shape[2:]
        t = t[:, :L].reshape((b, dilation, L) + rest)
        t = jnp.moveaxis(t, 1, 2)
        return t.reshape((b, s) + rest)

    return from_strided(o.reshape(n, Lp, h, hd)), from_strided(lse.reshape(n, Lp, h))


def dilated_attention(xn, k, v, w_q, q_norm, w_o):
    b, s, _ = xn.shape
    q = (xn @ w_q).reshape(b, s, N_HEADS, HEAD_DIM)
    q = rope(rms_norm(q, q_norm)).astype(F32)
    outs, lses = [], []
    for window, dilation in DILATED_BRANCHES:
        o_i, lse_i = dilated_branch(q, k, v, window, dilation)
        outs.append(o_i)
        lses.append(lse_i)
    alpha = jax.nn.softmax(jnp.stack(lses, axis=0), axis=0)[..., None]
    o = jnp.sum(alpha * jnp.stack(outs, axis=0), axis=0)
    return o.reshape(b, s, ATTN_WIDTH).astype(xn.dtype) @ w_o


def setup_inputs(seed: int = 0) -> dict:
    key = jax.random.key(seed)
    ks = jax.random.split(key, 24)

    def w(k, shape, fan_in):
        return jax.random.normal(k, shape, F32) * (fan_in ** -0.5)

    def gain(k, shape, noise=0.02):
        return jnp.ones(shape, F32) + noise * jax.random.normal(k, shape, F32)

    return {
        "x": jax.random.normal(ks[0], (BATCH, SEQ, D_MODEL), F32),
        "p": jax.random.normal(ks[1], (DEPTH, BATCH, SEQ, PLE_DIM), F32),
        "pool_norm": gain(ks[2], (N_A_LAYERS, D_MODEL)),
        "pool_w": w(ks[3], (N_A_LAYERS, POOL_GROUPS, POOL_GROUP_DIM, POOL_GROUP_DIM), POOL_GROUP_DIM),
        "pool_scale": gain(ks[4], (N_A_LAYERS, D_MODEL), 0.1),
        "kv_norm": gain(ks[5], (D_MODEL,)),
        "w_kv": w(ks[6], (D_MODEL, 2 * ATTN_WIDTH), D_MODEL),
        "k_norm": gain(ks[7], (HEAD_DIM,)),
        "attn_norm": gain(ks[8], (N_B_LAYERS, D_MODEL)),
        "w_q": w(ks[9], (N_B_LAYERS, D_MODEL, ATTN_WIDTH), D_MODEL),
        "q_norm": gain(ks[10], (N_B_LAYERS, HEAD_DIM)),
        "w_o": w(ks[11], (N_B_LAYERS, ATTN_WIDTH, D_MODEL), ATTN_WIDTH),
        "ffn_norm": gain(ks[12], (DEPTH, D_MODEL)),
        "dense_w_gate_up": w(ks[13], (N_DENSE_LAYERS, D_MODEL, 2 * D_FF), D_MODEL),
        "dense_w_down": w(ks[14], (N_DENSE_LAYERS, D_FF, D_MODEL), D_FF),
        "moe_router": w(ks[15], (N_MOE_LAYERS, D_MODEL, N_EXPERTS), D_MODEL),
        "moe_w_gate_up": w(ks[16], (N_MOE_LAYERS, N_EXPERTS, D_MODEL, 2 * D_FF_EXPERT), D_MODEL),
        "moe_w_down": w(ks[17], (N_MOE_LAYERS, N_EXPERTS, D_FF_EXPERT, D_MODEL), D_FF_EXPERT),
        "ple_w": w(ks[18], (DEPTH, PLE_DIM, D_MODEL), PLE_DIM),
        "ple_norm": gain(ks[19], (DEPTH, D_MODEL)),
        "ple_gate_w": w(ks[20], (DEPTH, D_MODEL, D_MODEL), D_MODEL),
    }


def reference(x, p, pool_norm, pool_w, pool_scale, kv_norm, w_kv, k_norm, attn_norm, w_q,
              q_norm, w_o, ffn_norm, dense_w_gate_up, dense_w_down, moe_router,
              moe_w_gate_up, moe_w_down, ple_w, ple_norm, ple_gate_w):
    h = x
    k_shared = None
    v_shared = None
    for i in range(DEPTH):
        if i < N_A_LAYERS:
            a = i
            h = h + multiscale_pool(rms_norm(h, pool_norm[a]), pool_w[a], pool_scale[a])
        else:
            bi = i - N_A_LAYERS
            h = h + dilated_attention(rms_norm(h, attn_norm[bi]), k_shared, v_shared,
                                      w_q[bi], q_norm[bi], w_o[bi])
        hn = rms_norm(h, ffn_norm[i])
        if i % 2 == 0:
            h = h + swiglu(hn, dense_w_gate_up[i // 2], dense_w_down[i // 2])
        else:
            h = h + moe_swiglu(hn, moe_router[i // 2], moe_w_gate_up[i // 2], moe_w_down[i // 2])
        h = per_layer_embedding(h, p[i], ple_w[i], ple_norm[i], ple_gate_w[i])
        if i == N_A_LAYERS - 1:
            k_shared, v_shared = shared_kv(h, kv_norm, w_kv, k_norm)
    return h
```

```python
import numpy as np
import ml_dtypes
import concourse.bass as bass
import concourse.mybir as mybir
from concourse.bass_utils import run_bass_kernel_spmd

F32 = mybir.dt.float32
BF16 = mybir.dt.bfloat16
AF = mybir.ActivationFunctionType
ALU = mybir.AluOpType

D = 4096
KC = 32
T = 1024
TH = 512
HALO = 16
TX = T + HALO
DFF = 11008
DFE = 3584
NE = 8
NH = 32
HD = 128
S = 2048
PW = 256
EPS = 1e-6
DENSE_GROUPS = (22, 22, 21, 21)
NCORES = 8

G_POOLN, G_POOLS, G_FFN0, G_PLE0, G_KVN, G_ATTN, G_FFN1, G_PLE1 = range(8)
NG = 8


class Sem:
    __slots__ = ("h", "n", "name")

    def __init__(self, h, name):
        self.h = h
        self.n = 0
        self.name = name


class Slot:
    def __init__(self, buf, sem=None):
        self.buf = buf
        self.rel = []
        self.sem = sem
        self.marker = None


class Marker:
    __slots__ = ("fn",)

    def __init__(self):
        self.fn = None


class Ring:
    def __init__(self, slots):
        self.slots = slots
        self.i = 0

    def next(self):
        s = self.slots[self.i % len(self.slots)]
        self.i += 1
        return s


class Bld:
    ENG = ("pe", "act", "dve", "pool", "sp")

    def __init__(self):
        self.nc = bass.Bass("TRN2", target_bir_lowering=False)
        self.q = {e: [] for e in self.ENG}
        self.waited = {e: {} for e in self.ENG}
        self.sems = []
        self.sb_off = 16512
        self.sb_top = 229344
        self.nsb = 0

    def sem(self, name):
        s = Sem(self.nc.alloc_semaphore(name), name)
        self.sems.append(s)
        return s

    def sb(self, shape, dtype, off=None):
        nbytes = int(np.prod(shape[1:])) * (4 if dtype == F32 else 2)
        if off is None:
            off = self.sb_off
            self.sb_off += (nbytes + 31) // 32 * 32
            assert self.sb_off <= self.sb_top, f"SBUF overflow {self.sb_off}"
        self.nsb += 1
        return self.nc.alloc_sbuf_tensor_at(f"sb{self.nsb}", list(shape), dtype, offset=off)

    def op(self, eng, fn, waits=(), inc=None, amt=1, marker=None):
        need = {}
        for ev in waits:
            if ev is None:
                continue
            s, v = ev
            if v > need.get(s, (None, 0))[1]:
                need[s] = (s, v)
        wl = []
        wd = self.waited[eng]
        for s, v in need.values():
            if marker is not None:
                wl.append((s.h, v))
            elif wd.get(s, 0) < v:
                wd[s] = v
                wl.append((s.h, v))
        ev = None
        if inc is not None:
            inc.n += amt
            ev = (inc, inc.n)
        inch = inc.h if inc is not None else None

        def thunk(e):
            for h, v in wl:
                e.wait_ge(h, v)
            ins = fn(e)
            if inch is not None:
                ins.then_inc(inch, amt)

        if marker is not None:
            marker.fn = thunk
        else:
            self.q[eng].append(thunk)
        return ev

    def dma(self, eng, out, in_, waits, sem, accum=False, marker=None):
        if accum:
            return self.op(eng, lambda e: e.dma_start(out=out, in_=in_, accum_op=ALU.add), waits, sem, 16)
        return self.op(eng, lambda e: e.dma_start(out=out, in_=in_), waits, sem, 16, marker=marker)

    def emit(self):
        with self.nc.Block() as blk:
            for name, deco in (("pe", blk.tensor), ("act", blk.scalar), ("dve", blk.vector),
                               ("pool", blk.gpsimd), ("sp", blk.sync)):
                q = self.q[name]

                def body(e, q=q):
                    for t in q:
                        if isinstance(t, Marker):
                            if t.fn is not None:
                                t.fn(e)
                        else:
                            t(e)

                deco(body)


def evs(*xs):
    out = []
    for x in xs:
        if x is None:
            continue
        if isinstance(x, list):
            out.extend(x)
        else:
            out.append(x)
    return out


class Prog:
    def __init__(self, which, upto=99):
        self.which = which
        self.upto = upto
        b = self.b = Bld()
        nc = b.nc
        self.dram = {}
        self.s_pe = b.sem("pe")
        self.s_act = b.sem("act")
        self.s_dve = b.sem("dve")
        self.s_pool = b.sem("pool")
        self.XN = b.sb([128, KC * T], BF16)
        self.BIG2 = b.sb([128, 28 * T], BF16)
        self.big2_off = b.sb_off - 28 * T * 2
        self.WB = Ring([Slot(b.sb([128, KC * PW], BF16), b.sem(f"wld{i}")) for i in range(2)])
        self.IN = Ring([Slot(b.sb([128, T], F32), b.sem(f"inld{i}")) for i in range(3)])
        self.OUT = Ring([Slot(b.sb([128, T], F32), b.sem(f"outst{i}")) for i in range(3)])
        self.SQ = Ring([Slot(b.sb([128, T], BF16)) for i in range(2)])
        self.SG = Ring([Slot(b.sb([128, T], BF16)) for i in range(2)])
        self.RSTD = b.sb([128, T], F32)
        self.rstd_rel = []
        self.GAINS = b.sb([128, NG * KC], F32)
        self.ONES = b.sb([128, 128], BF16)
        self.SMALL = b.sb([128, 64], F32)
        self.PS = [Slot(nc.alloc_psum_tensor(f"ps{i}", [128, T], F32)) for i in range(4)]
        self.ps_i = 0
        self.s_const = b.sem("const")
        self.s_misc = b.sem("misc")
        self.h_events = []
        self.s_misc2 = b.sem("misc2")
        self.ROUT = b.sb([128, KC * NE], F32)
        self.LG = b.sb([128, 64], F32)
        self.CMB = b.sb([128, 64], F32)
        self.GT = [b.sb([128, 64], F32) for i in range(7)]
        self.IDB = b.sb([128, 128], BF16)
        self.IDF = b.sb([8, 8], F32)
        self.LT = b.sb([8, T], F32)
        self.CB = Ring([Slot(b.sb([128, T], BF16)) for i in range(2)])
        self.DG = Ring([Slot(b.sb([128, 128], BF16)) for i in range(3)])
        self.xn_rel = []
        self.xn_ready = []
        self.big2_rel = []
        self.scr_off = self.big2_off
        self.nqk = 0
        self.nv = 0
        self.perm_ap = None

    def din(self, name, shape, dtype=F32):
        t = self.b.nc.dram_tensor(name, list(shape), dtype, kind="ExternalInput")
        self.dram[name] = t
        return t.ap()

    def dout(self, name, shape, dtype=F32):
        t = self.b.nc.dram_tensor(name, list(shape), dtype, kind="ExternalOutput")
        self.dram[name] = t
        return t.ap()

    def wb_load(self, pan, kcp):
        b = self.b
        w = self.WB.next()
        ld = b.dma("pool", w.buf[:, :kcp * PW], pan, evs(w.rel), w.sem, marker=w.marker)
        w.marker = None
        return w, ld

    def wb_done(self, w, pe_ev):
        w.rel = [pe_ev]
        w.marker = Marker()
        self.b.q["pool"].append(w.marker)

    def ps_next(self, subset=(0, 1, 2, 3)):
        s = self.PS[subset[self.ps_i % len(subset)]]
        self.ps_i += 1
        return s

    def consts(self, gains_ap, small_ap):
        b = self.b
        ev1 = b.dma("sp", self.GAINS[:], gains_ap, [], self.s_const)
        ev3 = b.dma("sp", self.SMALL[:], small_ap, [], self.s_const)
        ev2 = b.op("dve", lambda e: e.memset(self.ONES[:], 1.0), [], self.s_dve)
        self.const_ev = [ev3, ev2]

    def gain(self, gi, kc):
        return self.GAINS[:, gi * KC + kc: gi * KC + kc + 1]

    def norm_phase(self, hT, gi, extra_waits=()):
        b = self.b
        dep = evs(self.h_events, list(extra_waits), self.const_ev)
        ps = self.ps_next()
        pe_ev = None
        for kc in range(KC):
            sl = self.IN.next()
            ld = b.dma("sp", sl.buf[:], hT[kc * 128:(kc + 1) * 128, :], evs(sl.rel, dep), sl.sem)
            sq = self.SQ.next()
            a = b.op("act", lambda e, i=sl.buf, o=sq.buf: e.activation(out=o[:], in_=i[:], func=AF.Square),
                     evs(ld, sq.rel), self.s_act)
            sl.rel = [a]
            for th in range(2):
                pe_ev = b.op("pe", lambda e, o=ps.buf, r=sq.buf, th=th, kc=kc: e.matmul(
                    o[:, th * TH:(th + 1) * TH], lhsT=self.ONES[:], rhs=r[:, th * TH:(th + 1) * TH],
                    start=(kc == 0), stop=(kc == KC - 1)),
                    evs(a, ps.rel if kc == 0 else None), self.s_pe)
            sq.rel = [pe_ev]
        d1 = b.op("act", lambda e, i=ps.buf: e.activation(out=self.RSTD[:], in_=i[:], func=AF.Sqrt,
                                                           bias=self.SMALL[:, 0:1], scale=1.0 / D),
                  evs(pe_ev, self.rstd_rel, self.const_ev), self.s_act)
        ps.rel = [d1]
        d2 = b.op("dve", lambda e: e.reciprocal(out=self.RSTD[:], in_=self.RSTD[:]), [d1], self.s_dve)
        last = None
        for kc in range(KC):
            sl = self.IN.next()
            ld = b.dma("sp", sl.buf[:], hT[kc * 128:(kc + 1) * 128, :], evs(sl.rel, dep), sl.sem)
            last = b.op("dve", lambda e, i=sl.buf, kc=kc: e.scalar_tensor_tensor(
                out=self.XN[:, kc * T:(kc + 1) * T], in0=i[:], scalar=self.gain(gi, kc), in1=self.RSTD[:],
                op0=ALU.mult, op1=ALU.mult), evs(ld, d2, self.xn_rel), self.s_dve)
            sl.rel = [last]
        self.rstd_rel = [last]
        self.xn_ready = [last]

    def linear(self, panels, kcp, rhs, rhs_ready, epilogue, nn_per_panel=2):
        b = self.b
        last_pe = None
        for pi, pan in enumerate(panels):
            w, ld = self.wb_load(pan, kcp)
            for nn in range(nn_per_panel):
                ps = self.ps_next()
                ev = None
                for kc in range(kcp):
                    for th in range(2):
                        first = (kc == 0 and th == 0)
                        lastmm = (kc == kcp - 1 and th == 1)
                        ev = b.op("pe", lambda e, o=ps.buf, wb=w.buf, kc=kc, nn=nn, th=th: e.matmul(
                            o[:, th * TH:(th + 1) * TH], lhsT=wb[:, kc * PW + nn * 128: kc * PW + (nn + 1) * 128],
                            rhs=rhs(kc, th), start=(kc == 0), stop=(kc == kcp - 1)),
                            evs(ld, ps.rel, rhs_ready) if first else (), self.s_pe if lastmm else None)
                last_pe = ev
                if nn == nn_per_panel - 1:
                    self.wb_done(w, last_pe)
                epilogue(pi, nn, ps, ev)
        return last_pe

    def xn_rhs(self, kc, th):
        return self.XN[:, kc * T + th * TH: kc * T + (th + 1) * TH]

    def big2_rhs(self, kc, th):
        return self.BIG2[:, kc * T + th * TH: kc * T + (th + 1) * TH]

    def accum_out(self, hT, n, ps, ev, eng):
        b = self.b
        o = self.OUT.next()
        if eng == "act":
            c = b.op("act", lambda e, i=ps.buf, ob=o.buf: e.activation(out=ob[:], in_=i[:], func=AF.Copy),
                     evs(ev, o.rel), self.s_act)
        else:
            c = b.op("dve", lambda e, i=ps.buf, ob=o.buf: e.tensor_copy(out=ob[:], in_=i[:]),
                     evs(ev, o.rel), self.s_dve)
        ps.rel = [c]
        st = b.dma("pool", hT[n * 128:(n + 1) * 128, :], o.buf[:], [c], o.sem, accum=True)
        o.rel = [st]
        self.h_events = [s.rel[0] for s in self.OUT.slots if s.rel]
        return st

    def ffn_group(self, hT, gu_panels, dn_panels, fc, comb=None):
        b = self.b
        state = {}
        act_evs = []

        def gu_epi(pi, nn, ps, ev):
            if nn == 0:
                state["g"] = (ps, ev)
                return
            psg, evg = state["g"]
            sg = self.SG.next()
            a = b.op("act", lambda e, i=psg.buf, o=sg.buf: e.activation(out=o[:], in_=i[:], func=AF.Silu),
                     evs(evg, sg.rel), self.s_act)
            psg.rel = [a]
            if comb is None:
                d = b.op("dve", lambda e, i0=sg.buf, i1=ps.buf, pi=pi: e.tensor_tensor(
                    out=self.BIG2[:, pi * T:(pi + 1) * T], in0=i0[:], in1=i1[:], op=ALU.mult),
                    evs(a, ev, self.big2_rel), self.s_dve)
                ps.rel = [d]
                sg.rel = [d]
                act_evs.append(d)
            else:
                d = b.op("dve", lambda e, i0=sg.buf, i1=ps.buf: e.tensor_tensor(
                    out=i0[:], in0=i0[:], in1=i1[:], op=ALU.mult), evs(a, ev), self.s_dve)
                ps.rel = [d]
                g = b.op("pool", lambda e, i0=sg.buf, pi=pi: e.tensor_tensor(
                    out=self.BIG2[:, pi * T:(pi + 1) * T], in0=i0[:], in1=comb[0][:], op=ALU.mult),
                    evs(d, self.big2_rel, comb[1]), self.s_pool)
                sg.rel = [g]
                act_evs.append(g)

        self.linear(gu_panels, KC, self.xn_rhs, self.xn_ready, gu_epi)
        ready = act_evs[-3:]

        def dn_epi(pi, nn, ps, ev):
            self.accum_out(hT, pi * 2 + nn, ps, ev, "act" if nn == 0 else "dve")

        last = self.linear(dn_panels, fc, self.big2_rhs, ready, dn_epi)
        self.big2_rel = [last]
        self.xn_rel = [last]


    def scratch(self, shape, dtype):
        nbytes = int(np.prod(shape[1:])) * (4 if dtype == F32 else 2)
        off = self.scr_off
        self.scr_off += (nbytes + 31) // 32 * 32
        assert self.scr_off <= self.big2_off + 28 * T * 2, "scratch overflow"
        return self.b.sb(shape, dtype, off=off)

    def scratch_reset(self):
        self.scr_off = self.big2_off

    def mixer_phase(self, xT2, tk, hT, poolw, icnt_ap):
        b = self.b
        c0 = HALO + T * tk
        xe = xT2[:, c0 - HALO: c0 + T]
        self.scratch_reset()
        XE = Ring([Slot(self.scratch([128, TX], F32), b.sem(f"xe{tk}{i}")) for i in range(2)])
        XNF = Ring([Slot(self.scratch([128, TX], F32)) for i in range(2)])
        SA = Ring([Slot(self.scratch([128, TX], F32)) for i in range(2)])
        SB = Ring([Slot(self.scratch([128, TX], F32)) for i in range(2)])
        RSX = self.scratch([128, TX], F32)
        SQX = Ring([Slot(self.scratch([128, TX], BF16)) for i in range(2)])
        ICNT = self.scratch([128, 4 * 16], F32)
        T16 = Ring([Slot(self.scratch([128, 16], F32)) for i in range(2)])
        dep = evs(self.big2_rel, self.const_ev)
        ic = b.dma("sp", ICNT[:], icnt_ap, dep, self.s_misc)
        ps = self.ps_next()
        ps2 = self.ps_next()
        pieces = [(0, HALO, ps2, 0), (HALO, HALO + TH, ps, 0), (HALO + TH, TX, ps, TH)]
        pe_ev = None
        for kc in range(KC):
            sl = XE.next()
            ld = b.dma("sp", sl.buf[:], xe[kc * 128:(kc + 1) * 128, :], evs(sl.rel, dep), sl.sem)
            sq = SQX.next()
            a = b.op("act", lambda e, i=sl.buf, o=sq.buf: e.activation(out=o[:], in_=i[:], func=AF.Square),
                     evs(ld, sq.rel), self.s_act)
            sl.rel = [a]
            for (lo, hi, pp, po) in pieces:
                pe_ev = b.op("pe", lambda e, o=pp.buf, r=sq.buf, lo=lo, hi=hi, po=po, kc=kc: e.matmul(
                    o[:, po:po + hi - lo], lhsT=self.ONES[:], rhs=r[:, lo:hi], start=(kc == 0), stop=(kc == KC - 1)),
                    evs(a, ps.rel if kc == 0 else None, ps2.rel if kc == 0 else None), self.s_pe)
            sq.rel = [pe_ev]
        d1 = b.op("act", lambda e: e.activation(out=RSX[:, HALO:TX], in_=ps.buf[:], func=AF.Sqrt,
                                                bias=self.SMALL[:, 0:1], scale=1.0 / D), evs(pe_ev), self.s_act)
        d1b = b.op("act", lambda e: e.activation(out=RSX[:, 0:HALO], in_=ps2.buf[:, 0:HALO], func=AF.Sqrt,
                                                 bias=self.SMALL[:, 0:1], scale=1.0 / D), evs(pe_ev), self.s_act)
        ps.rel = [d1]
        ps2.rel = [d1b]
        d2 = b.op("dve", lambda e: e.reciprocal(out=RSX[:], in_=RSX[:]), [d1, d1b], self.s_dve)
        last = None
        for kc in range(KC):
            g = kc // 8
            w = 2 << g
            sl = XE.next()
            ld = b.dma("sp", sl.buf[:], xe[kc * 128:(kc + 1) * 128, :], evs(sl.rel, dep), sl.sem)
            xn = XNF.next()
            n1 = b.op("dve", lambda e, i=sl.buf, o=xn.buf, kc=kc: e.scalar_tensor_tensor(
                out=o[:], in0=i[:], scalar=self.gain(G_POOLN, kc), in1=RSX[:], op0=ALU.mult, op1=ALU.mult),
                evs(ld, d2, xn.rel), self.s_dve)
            sl.rel = [n1]
            cur, cur_ev = xn.buf, n1
            used = []
            for k in range(1, g + 2):
                sh = 1 << (k - 1)
                lo = (1 << k) - 1
                dst = (SA if k % 2 == 1 else SB).next()
                used.append(dst)
                cur_ev = b.op("pool", lambda e, o=dst.buf, c=cur, lo=lo, sh=sh: e.tensor_tensor(
                    out=o[:, lo:TX], in0=c[:, lo:TX], in1=c[:, lo - sh:TX - sh], op=ALU.add),
                    evs(cur_ev, dst.rel), self.s_pool)
                cur = dst.buf
            m = b.op("dve", lambda e, s_=cur, x_=xn.buf, kc=kc, w=w: e.scalar_tensor_tensor(
                out=self.XN[:, kc * T:(kc + 1) * T], in0=s_[:, HALO:TX], scalar=1.0 / w, in1=x_[:, HALO:TX],
                op0=ALU.mult, op1=ALU.subtract), evs(cur_ev, self.xn_rel), self.s_dve)
            t16 = T16.next()
            f1 = b.op("dve", lambda e, s_=cur, o=t16.buf, g=g: e.tensor_tensor(
                out=o[:], in0=s_[:, HALO:HALO + 16], in1=ICNT[:, g * 16:(g + 1) * 16], op=ALU.mult),
                evs(m, ic, t16.rel), self.s_dve)
            last = b.op("dve", lambda e, t_=t16.buf, x_=xn.buf, kc=kc: e.tensor_tensor(
                out=self.XN[:, kc * T:kc * T + 16], in0=t_[:], in1=x_[:, HALO:HALO + 16], op=ALU.subtract),
                evs(f1), self.s_dve)
            t16.rel = [last]
            xn.rel = [last]
            for u in used:
                u.rel = [last]
        ready = [last]
        lastpe = None
        for g in range(4):
            def epi(pi, nn, ps_, ev, g=g):
                n = 8 * g + 2 * pi + nn
                o = self.OUT.next()
                ld_ = b.dma("sp", o.buf[:], xe[n * 128:(n + 1) * 128, HALO:TX], evs(o.rel), o.sem)
                c = b.op("dve", lambda e, i=ps_.buf, ob=o.buf, n=n: e.scalar_tensor_tensor(
                    out=ob[:], in0=i[:], scalar=self.gain(G_POOLS, n), in1=ob[:], op0=ALU.mult, op1=ALU.add),
                    evs(ev, ld_), self.s_dve)
                ps_.rel = [c]
                st = b.dma("sp", hT[n * 128:(n + 1) * 128, :], o.buf[:], [c], o.sem)
                o.rel = [st]
            lastpe = self.linear([poolw[g, j] for j in range(4)], 8,
                                 lambda kc, th, g=g: self.XN[:, (8 * g + kc) * T + th * TH:(8 * g + kc) * T + (th + 1) * TH],
                                 ready, epi)
        self.xn_rel = [lastpe]
        self.big2_rel = evs(last, lastpe)
        self.h_events = [s.rel[0] for s in self.OUT.slots if s.rel]

    def ple_phase(self, hT, gi, pT, plew, gate_panels):
        b = self.b
        self.norm_phase(hT, gi)
        self.scratch_reset()
        PT = self.scratch([128, 2 * T], BF16)
        PWt = self.scratch([128, 2 * D], BF16)
        lds = []
        for kc in range(2):
            lds.append(b.dma("pool", PT[:, kc * T:(kc + 1) * T], pT[kc * 128:(kc + 1) * 128, :], evs(self.big2_rel), self.s_misc))
            lds.append(b.dma("pool", PWt[:, kc * D:(kc + 1) * D], plew[kc * 128:(kc + 1) * 128, :], evs(self.big2_rel), self.s_misc))
        lds = [lds[-1]]
        state = {"pe": None}

        def epi(pi, nn, ps, ev):
            n = 2 * pi + nn
            ps2 = self.ps_next()
            ev2 = None
            for kc in range(2):
                for th in range(2):
                    ev2 = b.op("pe", lambda e, o=ps2.buf, kc=kc, th=th, n=n: e.matmul(
                        o[:, th * TH:(th + 1) * TH], lhsT=PWt[:, kc * D + n * 128: kc * D + (n + 1) * 128],
                        rhs=PT[:, kc * T + th * TH: kc * T + (th + 1) * TH], start=(kc == 0), stop=(kc == 1)),
                        evs(lds, ps2.rel) if (kc == 0 and th == 0) else (),
                        self.s_pe if (kc == 1 and th == 1) else None)
            state["pe"] = ev2
            o = self.OUT.next()
            a = b.op("act", lambda e, i=ps.buf, ob=o.buf: e.activation(out=ob[:], in_=i[:], func=AF.Sigmoid),
                     evs(ev, o.rel), self.s_act)
            ps.rel = [a]
            d = b.op("dve", lambda e, i=ps2.buf, ob=o.buf: e.tensor_tensor(out=ob[:], in0=ob[:], in1=i[:], op=ALU.mult),
                     evs(a, ev2), self.s_dve)
            ps2.rel = [d]
            st = b.dma("pool", hT[n * 128:(n + 1) * 128, :], o.buf[:], [d], o.sem, accum=True)
            o.rel = [st]

        last = self.linear(gate_panels, KC, self.xn_rhs, self.xn_ready, epi)
        self.xn_rel = [last]
        self.big2_rel = evs(last, state["pe"])
        self.h_events = [s.rel[0] for s in self.OUT.slots if s.rel]

    def qk_phase(self, panels, gcol, rope_ap, dstT, col0):
        b = self.b
        self.scratch_reset()
        COS = self.scratch([128, T], F32)
        SIN = self.scratch([128, T], F32)
        PERM = self.scratch([128, 128], BF16)
        RS = [Slot(self.scratch([128, T], F32)) for i in range(2)]
        KN = [Slot(self.scratch([128, T], F32)) for i in range(2)]
        KNB = [Slot(self.scratch([128, T], BF16)) for i in range(2)]
        T1 = [Slot(self.scratch([128, T], F32)) for i in range(2)]
        T2 = [Slot(self.scratch([128, T], F32)) for i in range(2)]
        KO = [Slot(self.scratch([128, T], BF16), b.sem(f"ko{self.nqk}{i}")) for i in range(2)]
        self.nqk += 1
        dep = evs(self.big2_rel)
        l1 = b.dma("sp", COS[:], rope_ap[0], dep, self.s_misc)
        l2 = b.dma("sp", SIN[:], rope_ap[1], dep, self.s_misc)
        l3 = b.dma("pool", PERM[:], self.perm_ap, dep, self.s_misc2)
        tabs = [l2, l3]
        psK = [self.PS[0], self.PS[1]]
        psS, psR = self.PS[2], self.PS[3]
        st = {}
        wslot = None
        ld = None
        lastpe = None
        last_evs = []
        SPLIT = 6
        for i in range(NH + 2):
            ps = None
            if i < NH:
                pi, nn = i // 2, i % 2
                if nn == 0:
                    wslot, ld = self.wb_load(panels[pi], KC)
                ps = psK[i % 2]

            def emit_main(lo, hi, ps=ps, i=i):
                ev = None
                nn = i % 2
                for kc in range(lo, hi):
                    for th in range(2):
                        first = (kc == 0 and th == 0)
                        lastmm = (kc == KC - 1 and th == 1)
                        ev = b.op("pe", lambda e, o=ps.buf, wb=wslot.buf, kc=kc, nn=nn, th=th: e.matmul(
                            o[:, th * TH:(th + 1) * TH], lhsT=wb[:, kc * PW + nn * 128: kc * PW + (nn + 1) * 128],
                            rhs=self.xn_rhs(kc, th), start=(kc == 0), stop=(kc == KC - 1)),
                            evs(ld, ps.rel, self.xn_ready) if first else (), self.s_pe if lastmm else None)
                return ev

            if i < NH:
                emit_main(0, SPLIT)
            if i >= 2:
                j = i - 2
                s_ = st[j]
                p2 = None
                for th in range(2):
                    p2 = b.op("pe", lambda e, r=s_["knb"].buf, th=th: e.matmul(
                        psR.buf[:, th * TH:(th + 1) * TH], lhsT=PERM[:], rhs=r[:, th * TH:(th + 1) * TH],
                        start=True, stop=True), evs(s_["a3"], psR.rel, tabs) if th == 0 else (),
                        self.s_pe if th == 1 else None)
                s_["knb"].rel = [p2]
                t2, ko = T2[j % 2], KO[j % 2]
                d3 = b.op("dve", lambda e, o=t2.buf: e.tensor_tensor(out=o[:], in0=psR.buf[:], in1=SIN[:], op=ALU.mult),
                          evs(p2, t2.rel, tabs), self.s_dve)
                psR.rel = [d3]
                g2 = b.op("pool", lambda e, a_=s_["t1"].buf, b_=t2.buf, o=ko.buf: e.tensor_tensor(
                    out=o[:], in0=a_[:], in1=b_[:], op=ALU.add), evs(s_["g1"], d3, ko.rel), self.s_pool)
                s_["t1"].rel = [g2]
                t2.rel = [g2]
                stv = b.dma("sp", dstT[j, :, col0:col0 + T], ko.buf[:], [g2], ko.sem)
                ko.rel = [stv]
                last_evs = [k.rel[0] for k in KO if k.rel]
                del st[j]
            if 1 <= i <= NH:
                j = i - 1
                s_ = st[j]
                p1 = None
                for th in range(2):
                    p1 = b.op("pe", lambda e, r=s_["sq"].buf, th=th: e.matmul(
                        psS.buf[:, th * TH:(th + 1) * TH], lhsT=self.ONES[:], rhs=r[:, th * TH:(th + 1) * TH],
                        start=True, stop=True), evs(s_["a1"], psS.rel) if th == 0 else (), self.s_pe if th == 1 else None)
                s_["sq"].rel = [p1]
                rs, kn, knb = RS[j % 2], KN[j % 2], KNB[j % 2]
                a2 = b.op("act", lambda e, o=rs.buf: e.activation(out=o[:], in_=psS.buf[:], func=AF.Sqrt,
                                                                  bias=self.SMALL[:, 0:1], scale=1.0 / HD),
                          evs(p1, rs.rel), self.s_act)
                psS.rel = [a2]
                d1 = b.op("dve", lambda e, o=rs.buf: e.reciprocal(out=o[:], in_=o[:]), [a2], self.s_dve)
                d2 = b.op("dve", lambda e, i_=s_["ps"].buf, o=kn.buf, r=rs.buf: e.scalar_tensor_tensor(
                    out=o[:], in0=i_[:], scalar=self.SMALL[:, gcol:gcol + 1], in1=r[:], op0=ALU.mult, op1=ALU.mult),
                    evs(d1, s_["ev"], kn.rel, self.const_ev), self.s_dve)
                s_["ps"].rel = [d2]
                rs.rel = [d2]
                a3 = b.op("act", lambda e, i_=kn.buf, o=knb.buf: e.activation(out=o[:], in_=i_[:], func=AF.Copy),
                          evs(d2, knb.rel), self.s_act)
                t1 = T1[j % 2]
                g1 = b.op("pool", lambda e, i_=kn.buf, o=t1.buf: e.tensor_tensor(out=o[:], in0=i_[:], in1=COS[:], op=ALU.mult),
                          evs(d2, tabs, t1.rel), self.s_pool)
                kn.rel = [a3, g1]
                s_.update(a3=a3, g1=g1, knb=knb, t1=t1)
            if i < NH:
                ev = emit_main(SPLIT, KC)
                lastpe = ev
                if nn == 1:
                    self.wb_done(wslot, ev)
                sq = self.SQ.next()
                a1 = b.op("act", lambda e, i_=ps.buf, o=sq.buf: e.activation(out=o[:], in_=i_[:], func=AF.Square),
                          evs(ev, sq.rel), self.s_act)
                st[i] = dict(ps=ps, ev=ev, sq=sq, a1=a1)
        self.xn_rel = [lastpe]
        self.big2_rel = evs(last_evs, lastpe)
        return last_evs

    def v_phase(self, panels, Vd, row0):
        b = self.b
        VST = Ring([Slot(self.scratch([128, T], BF16), b.sem(f"vst{self.nv}{i}")) for i in range(2)])
        self.nv += 1
        lastpe = None
        cnt = 0
        for pi, pan in enumerate(panels):
            w, ld = self.wb_load(pan, KC)
            for hh in range(2):
                ps = self.ps_next()
                ev = None
                for tt in range(4):
                    tok = (4 * hh + tt) * 128
                    for kc in range(KC):
                        first = (tt == 0 and kc == 0)
                        lastmm = (tt == 3 and kc == KC - 1)
                        ev = b.op("pe", lambda e, o=ps.buf, wb=w.buf, kc=kc, tt=tt, tok=tok: e.matmul(
                            o[:, tt * PW:(tt + 1) * PW], lhsT=self.XN[:, kc * T + tok: kc * T + tok + 128],
                            rhs=wb[:, kc * PW:(kc + 1) * PW], start=(kc == 0), stop=(kc == KC - 1)),
                            evs(ld, ps.rel, self.xn_ready) if first else (), self.s_pe if lastmm else None)
                lastpe = ev
                if hh == 1:
                    self.wb_done(w, lastpe)
                v = VST.next()
                if cnt % 2 == 0:
                    c = b.op("act", lambda e, i=ps.buf, o=v.buf: e.activation(out=o[:], in_=i[:], func=AF.Copy),
                             evs(ev, v.rel), self.s_act)
                else:
                    c = b.op("dve", lambda e, i=ps.buf, o=v.buf: e.tensor_copy(out=o[:], in_=i[:]),
                             evs(ev, v.rel), self.s_dve)
                cnt += 1
                ps.rel = [c]
                r0 = row0 + 4 * hh * 128
                dst = Vd[r0:r0 + 512, pi * PW:(pi + 1) * PW].rearrange("(tt p) c -> p tt c", p=128)
                src = v.buf[:, :].rearrange("p (tt c) -> p tt c", c=PW)
                stv = b.dma("sp", dst, src, [c], v.sem)
                v.rel = [stv]
        self.xn_rel = [lastpe]
        outs = [s.rel[0] for s in VST.slots if s.rel]
        self.big2_rel = evs(self.big2_rel, outs, lastpe)
        return outs

    def attn_phase(self, KT, Vd, QT, mask_ap, valid_ap, kv_ready):
        b = self.b
        self.scratch_reset()
        MSK = self.scratch([128, 19 * 128], BF16)
        VAL = self.scratch([128, 16], F32)
        KTh = Ring([Slot(self.scratch([128, S], BF16), b.sem(f"kth{i}")) for i in range(2)])
        Vh = Ring([Slot(self.scratch([128, 16 * 128], BF16), b.sem(f"vh{i}")) for i in range(2)])
        Qh = Ring([Slot(self.scratch([128, T], BF16), b.sem(f"qh{i}")) for i in range(2)])
        EB = Ring([Slot(self.scratch([128, TH], BF16)) for i in range(3)])
        PB = Ring([Slot(self.scratch([128, TH], BF16)) for i in range(3)])
        RD = Ring([Slot(self.scratch([128, TH], F32)) for i in range(2)])
        dep = evs(self.big2_rel, kv_ready)
        m1 = b.dma("pool", MSK[:], mask_ap, dep, self.s_misc2)
        m2 = b.dma("sp", VAL[:], valid_ap, dep, self.s_misc)
        psND = [self.PS[0], self.PS[1]]
        psS = [(self.PS[2], 0), (self.PS[2], 1), (self.PS[3], 0), (self.PS[3], 1)]
        srel = [list(self.PS[2].rel), list(self.PS[2].rel), list(self.PS[3].rel), list(self.PS[3].rel)]
        scale = 1.0 / float(np.sqrt(HD))
        si = 0
        nd_i = 0
        cnt = 0
        last = None
        lastpe = None
        for h in range(NH):
            k_, v_, q_ = KTh.next(), Vh.next(), Qh.next()
            lk = b.dma("sp", k_.buf[:], KT[h], evs(k_.rel, dep), k_.sem)
            lv = b.dma("sp", v_.buf[:, :].rearrange("p (j d) -> p j d", d=128),
                       Vd[:, h * 128:(h + 1) * 128].rearrange("(j p) d -> p j d", p=128), evs(v_.rel, dep), v_.sem)
            lq = b.dma("sp", q_.buf[:], QT[h], evs(q_.rel, dep), q_.sem)
            for c in range(2):
                nkb = 12 + 4 * c
                nd = psND[nd_i % 2]
                nd_i += 1
                sevs = {}

                def emit_s(jj):
                    nonlocal si
                    pslot, half = psS[si % 4]
                    idx = si % 4
                    si += 1
                    ev = b.op("pe", lambda e, o=pslot.buf, half=half, jj=jj, kb=k_.buf, qb=q_.buf, c=c: e.matmul(
                        o[:, half * TH:(half + 1) * TH], lhsT=kb[:, jj * 128:(jj + 1) * 128],
                        rhs=qb[:, c * TH:(c + 1) * TH], start=True, stop=True),
                        evs(lk, lq, srel[idx]), self.s_pe)
                    sevs[jj] = (pslot, half, idx, ev)

                emit_s(0)
                emit_s(1)
                for jj in range(nkb):
                    if jj + 2 < nkb:
                        emit_s(jj + 2)
                    pslot, half, idx, sev = sevs[jj]
                    eb = EB.next()
                    a = b.op("act", lambda e, i=pslot.buf, half=half, o=eb.buf: e.activation(
                        out=o[:], in_=i[:, half * TH:(half + 1) * TH], func=AF.Exp, scale=scale),
                        evs(sev, eb.rel), self.s_act)
                    srel[idx] = [a]
                    pb = PB.next()
                    dd = 8 + 4 * c - jj
                    d = b.op("dve", lambda e, i=eb.buf, o=pb.buf, jj=jj, dd=dd: e.scalar_tensor_tensor(
                        out=o[:], in0=i[:], scalar=VAL[:, jj:jj + 1], in1=MSK[:, (dd + 3) * 128:(dd + 3) * 128 + TH],
                        op0=ALU.mult, op1=ALU.mult), evs(a, pb.rel, m1, m2), self.s_dve)
                    eb.rel = [d]
                    b.op("pe", lambda e, o=nd.buf, jj=jj, p=pb.buf, vb=v_.buf, nkb=nkb: e.matmul(
                        o[:, 0:TH], lhsT=vb[:, jj * 128:(jj + 1) * 128], rhs=p[:], start=(jj == 0), stop=(jj == nkb - 1)),
                        evs(d, lv, nd.rel if jj == 0 else None), None)
                    lastpe = b.op("pe", lambda e, o=nd.buf, jj=jj, p=pb.buf, nkb=nkb: e.matmul(
                        o[:, TH:2 * TH], lhsT=self.ONES[:], rhs=p[:], start=(jj == 0), stop=(jj == nkb - 1)),
                        (), self.s_pe)
                    pb.rel = [lastpe]
                rd = RD.next()
                r1 = b.op("dve", lambda e, i=nd.buf, o=rd.buf: e.reciprocal(out=o[:], in_=i[:, TH:2 * TH]),
                          evs(lastpe, rd.rel), self.s_dve)
                last = b.op("dve", lambda e, i=nd.buf, r=rd.buf, h=h, c=c: e.tensor_tensor(
                    out=self.XN[:, h * T + c * TH: h * T + (c + 1) * TH], in0=i[:, 0:TH], in1=r[:], op=ALU.mult),
                    evs(r1, self.xn_rel), self.s_dve)
                rd.rel = [last]
                nd.rel = [last]
            k_.rel = [lastpe]
            v_.rel = [lastpe]
            q_.rel = [lastpe]
        for idx in range(4):
            psS[idx][0].rel = evs(psS[idx][0].rel, srel[idx])
        self.xn_ready = [last]
        self.big2_rel = evs(lastpe, last)

    def simple_linear_accum(self, hT, panels):
        def epi(pi, nn, ps, ev):
            self.accum_out(hT, pi * 2 + nn, ps, ev, "act" if nn == 0 else "dve")
        last = self.linear(panels, KC, self.xn_rhs, self.xn_ready, epi)
        self.xn_rel = [last]

    def moe_phase(self, hT, gi, router_ap, ident_ap, moe_gu, moe_dn):
        b = self.b
        self.scratch_reset()
        ROUT, LG, IDB, CMB = self.ROUT, self.LG, self.IDB, self.CMB
        lr = b.dma("sp", ROUT[:], router_ap, evs(self.const_ev), self.s_misc)
        li = b.dma("pool", IDB[:], ident_ap, [], self.s_misc2)
        dep = evs(self.h_events, self.const_ev)
        ps = self.ps_next()
        pe_ev = None
        for kc in range(KC):
            sl = self.IN.next()
            ld = b.dma("sp", sl.buf[:], hT[kc * 128:(kc + 1) * 128, :], evs(sl.rel, dep), sl.sem)
            sq = self.SQ.next()
            a = b.op("act", lambda e, i=sl.buf, o=sq.buf: e.activation(out=o[:], in_=i[:], func=AF.Square),
                     evs(ld, sq.rel), self.s_act)
            sl.rel = [a]
            for th in range(2):
                pe_ev = b.op("pe", lambda e, o=ps.buf, r=sq.buf, th=th, kc=kc: e.matmul(
                    o[:, th * TH:(th + 1) * TH], lhsT=self.ONES[:], rhs=r[:, th * TH:(th + 1) * TH],
                    start=(kc == 0), stop=(kc == KC - 1)), evs(a, ps.rel if kc == 0 else None), self.s_pe)
            sq.rel = [pe_ev]
        d1 = b.op("act", lambda e, i=ps.buf: e.activation(out=self.RSTD[:], in_=i[:], func=AF.Sqrt,
                                                           bias=self.SMALL[:, 0:1], scale=1.0 / D),
                  evs(pe_ev, self.rstd_rel), self.s_act)
        ps.rel = [d1]
        d2 = b.op("dve", lambda e: e.reciprocal(out=self.RSTD[:], in_=self.RSTD[:]), [d1], self.s_dve)
        psl = self.ps_next()
        last = None
        lpe = None
        for kc in range(KC):
            sl = self.IN.next()
            ld = b.dma("sp", sl.buf[:], hT[kc * 128:(kc + 1) * 128, :], evs(sl.rel, dep), sl.sem)
            n1 = b.op("dve", lambda e, i=sl.buf, kc=kc: e.scalar_tensor_tensor(
                out=i[:], in0=i[:], scalar=self.gain(gi, kc), in1=self.RSTD[:], op0=ALU.mult, op1=ALU.mult),
                evs(ld, d2), self.s_dve)
            last = b.op("act", lambda e, i=sl.buf, kc=kc: e.activation(out=self.XN[:, kc * T:(kc + 1) * T], in_=i[:], func=AF.Copy),
                        evs(n1, self.xn_rel), self.s_act)
            for th in range(2):
                lpe = b.op("pe", lambda e, i=sl.buf, th=th, kc=kc: e.matmul(
                    psl.buf[0:8, th * TH:(th + 1) * TH], lhsT=ROUT[:, kc * 8:(kc + 1) * 8], rhs=i[:, th * TH:(th + 1) * TH],
                    start=(kc == 0), stop=(kc == KC - 1)),
                    evs(n1, lr, psl.rel if kc == 0 else None) if th == 0 else (), self.s_pe if th == 1 else None)
            sl.rel = [last, lpe]
        self.rstd_rel = [n1]
        self.xn_ready = [last]
        lt = b.op("dve", lambda e: e.tensor_copy(out=self.LT[:], in_=psl.buf[0:8, :]), [lpe], self.s_dve)
        psl.rel = [lt]
        lif = b.dma("sp", self.IDF[:], ident_ap[0:8, 0:8], [], self.s_misc)
        psl2 = self.ps_next()
        tp = None
        for tt in range(8):
            tp = b.op("pe", lambda e, tt=tt: e.matmul(
                psl2.buf[:, tt * 8:(tt + 1) * 8], lhsT=self.LT[0:8, tt * 128:(tt + 1) * 128], rhs=self.IDF[0:8, 0:8],
                start=True, stop=True), evs(lt, lif, psl2.rel) if tt == 0 else (), self.s_pe if tt == 7 else None)
        M1, M2, EQ, L2, NEG, EX, DEN = self.GT
        v3 = lambda t: t[:, 0:64].rearrange("p (a e) -> p a e", e=8)
        bc = lambda t: t[:, 0:8].unsqueeze(2).to_broadcast([128, 8, 8])
        c0 = b.op("dve", lambda e: e.tensor_copy(out=LG[:, 0:64], in_=psl2.buf[:, 0:64]), [tp], self.s_dve)
        psl2.rel = [c0]
        c1 = b.op("dve", lambda e: e.tensor_reduce(out=M1[:, 0:8], in_=v3(LG), axis=mybir.AxisListType.X, op=ALU.max), [c0], self.s_dve)
        c2 = b.op("dve", lambda e: e.tensor_tensor(out=v3(EQ), in0=v3(LG), in1=bc(M1), op=ALU.is_equal), [c1], self.s_dve)
        c3 = b.op("dve", lambda e: e.scalar_tensor_tensor(out=L2[:, 0:64], in0=EQ[:, 0:64], scalar=-1e30, in1=LG[:, 0:64],
                                                           op0=ALU.mult, op1=ALU.add), [c2], self.s_dve)
        c4 = b.op("dve", lambda e: e.tensor_reduce(out=M2[:, 0:8], in_=v3(L2), axis=mybir.AxisListType.X, op=ALU.max), [c3], self.s_dve)
        c5 = b.op("dve", lambda e: e.tensor_tensor(out=v3(EQ), in0=v3(LG), in1=bc(M2), op=ALU.is_ge), [c4], self.s_dve)
        c6 = b.op("dve", lambda e: e.tensor_tensor(out=v3(L2), in0=v3(LG), in1=bc(M1), op=ALU.subtract), [c5], self.s_dve)
        c7 = b.op("act", lambda e: e.activation(out=EX[:, 0:64], in_=L2[:, 0:64], func=AF.Exp), [c6], self.s_act)
        c8 = b.op("dve", lambda e: e.tensor_tensor(out=EX[:, 0:64], in0=EX[:, 0:64], in1=EQ[:, 0:64], op=ALU.mult), [c7], self.s_dve)
        c9 = b.op("dve", lambda e: e.tensor_reduce(out=DEN[:, 0:8], in_=v3(EX), axis=mybir.AxisListType.X, op=ALU.add), [c8], self.s_dve)
        c10 = b.op("dve", lambda e: e.reciprocal(out=DEN[:, 0:8], in_=DEN[:, 0:8]), [c9], self.s_dve)
        c11 = b.op("dve", lambda e: e.tensor_tensor(out=v3(CMB), in0=v3(EX), in1=bc(DEN), op=ALU.mult), [c10], self.s_dve)
        DG = self.DG
        for ex in range(NE):
            cb = self.CB.next()
            psc = self.ps_next()
            pe = None
            for tt in range(8):
                dg = DG.next()
                dv = b.op("dve", lambda e, o=dg.buf, tt=tt, ex=ex: e.tensor_scalar_mul(
                    out=o[:], in0=IDB[:], scalar1=CMB[:, tt * 8 + ex: tt * 8 + ex + 1]),
                    evs(c11, li, dg.rel), self.s_dve)
                pe = b.op("pe", lambda e, o=psc.buf, r=dg.buf, tt=tt: e.matmul(
                    o[:, tt * 128:(tt + 1) * 128], lhsT=self.ONES[:], rhs=r[:], start=True, stop=True),
                    evs(dv, psc.rel if tt == 0 else None), self.s_pe)
                dg.rel = [pe]
            cc = b.op("act", lambda e, i=psc.buf, o=cb.buf: e.activation(out=o[:], in_=i[:], func=AF.Copy),
                      evs(pe, cb.rel), self.s_act)
            psc.rel = [cc]
            self.ffn_group(hT, [moe_gu[ex, f] for f in range(28)], [moe_dn[ex, j] for j in range(D // PW)], 28,
                           comb=(cb.buf, cc))
            cb.rel = list(self.big2_rel)

    def finish(self, final_events):
        b = self.b
        b.op("sp", lambda e: e.nop(), evs(final_events), None)
        b.emit()
        return b.nc


def tile_w(w, pw=PW):
    K, N = w.shape
    kc = K // 128
    npan = N // pw
    return np.ascontiguousarray(w.reshape(kc, 128, npan, pw).transpose(2, 1, 0, 3)).reshape(npan, 128, kc * pw)


def tile_gu(w, dff):
    K = w.shape[0]
    g = w[:, :dff].reshape(K, dff // 128, 1, 128)
    u = w[:, dff:].reshape(K, dff // 128, 1, 128)
    return tile_w(np.concatenate([g, u], axis=2).reshape(K, 2 * dff))


def fm(v):
    return np.ascontiguousarray(v.reshape(-1, 128).T)


def build_full(upto=99, tiles=(0, 1), debug=False):
    P = Prog("F", upto)
    b = P.b
    nc = b.nc
    NP16 = D // PW
    xT2 = P.din("xT2", [D, HALO + 2 * T])
    gains = P.din("gains", [128, NG * KC])
    small = P.din("small", [128, 64])
    icnt = P.din("icnt", [2, 128, 64])
    poolw = P.din("poolw", [4, 4, 128, 8 * PW])
    hT = P.dout("hT", [D, T])
    hprev = nc.dram_tensor("hprev", [D, T], F32).ap()
    if debug:
        KT = P.dout("KTs", [NH, 128, S], BF16)
        Vd = P.dout("Vds", [S, D], BF16)
        QT = P.dout("QTs", [NH, 128, T], BF16)
    else:
        KT = nc.dram_tensor("KTs", [NH, 128, S], BF16).ap()
        Vd = nc.dram_tensor("Vds", [S, D], BF16).ap()
        QT = nc.dram_tensor("QTs", [NH, 128, T], BF16).ap()
    P.consts(gains, small)
    kv_events = []
    stage = 0

    def more():
        nonlocal stage
        stage += 1
        return stage <= upto

    done = False
    for tk in tiles:
        h = hprev if tk == 0 else hT
        if not more():
            done = True
            break
        P.mixer_phase(xT2, tk, h, poolw, icnt[tk])
        if not more():
            done = True
            break
        if tk == tiles[0]:
            wgu0 = P.din("wgu0", [DFF // 128, 128, KC * PW])
            wdn0 = [P.din(f"wdn0_{g}", [NP16, 128, fc * PW]) for g, fc in enumerate(DENSE_GROUPS)]
        P.norm_phase(h, G_FFN0)
        f0 = 0
        for g, fc in enumerate(DENSE_GROUPS):
            P.ffn_group(h, [wgu0[f0 + i] for i in range(fc)], [wdn0[g][j] for j in range(NP16)], fc)
            f0 += fc
        if not more():
            done = True
            break
        if tk == tiles[0]:
            p0T = P.din("p0T", [256, 2 * T])
            plew0 = P.din("plew0", [256, D])
            pleg0 = P.din("pleg0", [NP16, 128, KC * PW])
        P.ple_phase(h, G_PLE0, p0T[:, tk * T:(tk + 1) * T], plew0, [pleg0[j] for j in range(NP16)])
        if not more():
            done = True
            break
        if tk == tiles[0]:
            wk = P.din("wk", [NP16, 128, KC * PW])
            wv = P.din("wv", [NP16, 128, KC * PW])
            rope = P.din("rope", [2, 2, 128, T])
            P.perm_ap = P.din("perm", [128, 128])
        P.norm_phase(h, G_KVN)
        kst = P.qk_phase([wk[j] for j in range(NP16)], 1, rope[tk], KT, tk * T)
        vst = P.v_phase([wv[j] for j in range(NP16)], Vd, tk * T)
        kv_events += kst + vst
    fin = None
    while not done:
        if not more():
            break
        wq = P.din("wq", [NP16, 128, KC * PW])
        mask = P.din("mask", [128, 19 * 128])
        valid = P.din("valid", [128, 16])
        P.norm_phase(hT, G_ATTN)
        qst = P.qk_phase([wq[j] for j in range(NP16)], 2, rope[1], QT, 0)
        P.attn_phase(KT, Vd, QT, mask, valid, kv_events + qst)
        if not more():
            break
        wo = P.din("wo", [NP16, 128, KC * PW])
        P.simple_linear_accum(hT, [wo[j] for j in range(NP16)])
        if not more():
            break
        router = P.din("router", [128, KC * NE])
        ident = P.din("ident", [128, 128])
        moegu = P.din("moegu", [NE, DFE // 128, 128, KC * PW])
        moedn = P.din("moedn", [NE, NP16, 128, (DFE // 128) * PW])
        P.moe_phase(hT, G_FFN1, router, ident, moegu, moedn)
        if not more():
            break
        p1T = P.din("p1T", [256, T])
        plew1 = P.din("plew1", [256, D])
        pleg1 = P.din("pleg1", [NP16, 128, KC * PW])
        P.ple_phase(hT, G_PLE1, p1T, plew1, [pleg1[j] for j in range(NP16)])
        break
    if upto == 9:
        pass
    nc_ = P.finish(evs(P.h_events, kv_events, list(P.xn_ready)))
    return nc_, sorted(P.dram.keys())


def _mult(delta):
    m = np.zeros_like(delta, dtype=np.float32)
    m += ((delta >= 0) & (delta <= 128)).astype(np.float32)
    m += ((delta >= 0) & (delta % 4 == 0) & (delta <= 512)).astype(np.float32)
    m += ((delta >= 0) & (delta % 16 == 0) & (delta <= 2048)).astype(np.float32)
    return m


def host_inputs(inp, names, cores):
    x = inp["x"]
    p = inp["p"]
    shared = {}
    g = np.zeros((128, NG * KC), np.float32)
    for gi, v in ((G_POOLN, inp["pool_norm"][0]), (G_POOLS, inp["pool_scale"][0]), (G_FFN0, inp["ffn_norm"][0]),
                  (G_PLE0, inp["ple_norm"][0]), (G_KVN, inp["kv_norm"]), (G_ATTN, inp["attn_norm"][0]),
                  (G_FFN1, inp["ffn_norm"][1]), (G_PLE1, inp["ple_norm"][1])):
        g[:, gi * KC:(gi + 1) * KC] = fm(np.asarray(v, np.float32))
    shared["gains"] = g
    sm = np.zeros((128, 64), np.float32)
    sm[:, 0] = EPS
    sm[:, 1] = inp["k_norm"]
    sm[:, 2] = inp["q_norm"][0]
    shared["small"] = sm
    if "poolw" in names:
        shared["poolw"] = np.stack([tile_w(inp["pool_w"][0, gg]) for gg in range(4)])
    if "wgu0" in names:
        shared["wgu0"] = tile_gu(inp["dense_w_gate_up"][0], DFF)
        f0 = 0
        for gg, fc in enumerate(DENSE_GROUPS):
            shared[f"wdn0_{gg}"] = tile_w(inp["dense_w_down"][0][f0 * 128:(f0 + fc) * 128, :])
            f0 += fc
    for l in (0, 1):
        if f"plew{l}" in names:
            shared[f"plew{l}"] = np.ascontiguousarray(inp["ple_w"][l])
            shared[f"pleg{l}"] = tile_w(inp["ple_gate_w"][l])
    if "wk" in names:
        shared["wk"] = tile_w(inp["w_kv"][:, :D])
        shared["wv"] = tile_w(inp["w_kv"][:, D:])
        pm = np.zeros((128, 128), np.float32)
        for m in range(128):
            pm[(m + 64) % 128, m] = 1.0
        shared["perm"] = pm
    if "wq" in names:
        shared["wq"] = tile_w(inp["w_q"][0])
        kj = np.arange(128)[:, None]
        qi = np.arange(128)[None, :]
        strip = np.concatenate([_mult(128 * d + qi - kj) for d in range(-3, 16)], axis=1)
        shared["mask"] = np.ascontiguousarray(strip.astype(np.float32))
    if "wo" in names:
        shared["wo"] = tile_w(inp["w_o"][0])
    if "router" in names:
        r = inp["moe_router"][0]
        shared["router"] = np.ascontiguousarray(r.reshape(KC, 128, NE).transpose(1, 0, 2)).reshape(128, KC * NE)
        shared["ident"] = np.eye(128, dtype=np.float32)
        shared["moegu"] = np.stack([tile_gu(inp["moe_w_gate_up"][0, e], DFE) for e in range(NE)])
        shared["moedn"] = np.stack([tile_w(inp["moe_w_down"][0, e]) for e in range(NE)])
    half = HD // 2
    inv_freq = np.exp(-np.log(np.float32(10000.0)) * np.arange(half, dtype=np.float32) / np.float32(half)).astype(np.float32)
    in_maps = []
    for c in cores:
        bb, hf = c // 2, c % 2
        m = dict(shared)
        xt = np.zeros((D, HALO + 2 * T), np.float32)
        if hf == 1:
            xt[:, HALO:HALO + T] = x[bb, 0:T, :].T
        xt[:, HALO + T:] = x[bb, hf * T:(hf + 1) * T, :].T
        m["xT2"] = xt
        ic = np.zeros((2, 128, 64), np.float32)
        for gg in range(4):
            w = 2 << gg
            start = 1.0 / np.minimum(np.arange(16) + 1, w).astype(np.float32)
            ic[0, :, gg * 16:(gg + 1) * 16] = start
            ic[1, :, gg * 16:(gg + 1) * 16] = start if hf == 0 else np.float32(1.0 / w)
        m["icnt"] = ic
        if "p0T" in names:
            pt = np.zeros((256, 2 * T), np.float32)
            if hf == 1:
                pt[:, :T] = p[0, bb, 0:T, :].T
            pt[:, T:] = p[0, bb, hf * T:(hf + 1) * T, :].T
            m["p0T"] = pt
        if "p1T" in names:
            m["p1T"] = np.ascontiguousarray(p[1, bb, hf * T:(hf + 1) * T, :].T)
        if "rope" in names:
            rp = np.zeros((2, 2, 128, T), np.float32)
            for tk in range(2):
                pos0 = 0 if tk == 0 else hf * T
                ang = (np.arange(pos0, pos0 + T, dtype=np.float32)[:, None] * inv_freq[None, :]).astype(np.float32)
                cs = np.cos(ang).T.astype(np.float32)
                sn = np.sin(ang).T.astype(np.float32)
                rp[tk, 0] = np.concatenate([cs, cs], axis=0)
                rp[tk, 1] = np.concatenate([-sn, sn], axis=0)
            m["rope"] = rp
        if "valid" in names:
            v = np.ones((128, 16), np.float32)
            if hf == 0:
                v[:, :8] = 0.0
            m["valid"] = v
        in_maps.append({k: m[k] for k in names if k not in ("hT", "KTs", "Vds", "QTs")})
    return in_maps


_CACHE = {}


def kernel(**inputs):
    inp = {k: np.asarray(v) for k, v in inputs.items()}
    if "prog" not in _CACHE:
        _CACHE["prog"] = build_full()
    nc, names = _CACHE["prog"]
    cores = list(range(NCORES))
    in_maps = host_inputs(inp, names, cores)
    res = run_bass_kernel_spmd(nc, in_maps, core_ids=cores)
    out = np.empty((4, S, D), np.float32)
    for c in cores:
        bb, hf = c // 2, c % 2
        out[bb, hf * T:(hf + 1) * T, :] = res.results[c]["hT"].T
    return out


def build_test_ffn():
    P = Prog("T")
    b = P.b
    xT = P.din("xT", [D, T])
    gains = P.din("gains", [128, NG * KC])
    wgu = P.din("wgu", [DFF // 128, 128, KC * PW])
    wdn = [P.din(f"wdn{g}", [D // PW, 128, fc * PW]) for g, fc in enumerate(DENSE_GROUPS)]
    hT = P.dout("hT", [D, T])
    P.consts(gains)
    P.xn_rel = []
    P.big2_rel = []
    c = b.dma("sp", hT[:, :], xT[:, :], [], P.s_misc)
    P.h_events = [c]
    P.norm_phase(hT, G_FFN0)
    f0 = 0
    for g, fc in enumerate(DENSE_GROUPS):
        if g >= P.upto:
            break
        P.ffn_group(hT, [wgu[f0 + i] for i in range(fc)], [wdn[g][j] for j in range(D // PW)], fc)
        f0 += fc
    return P.finish(P.h_events)
```

```python
import numpy as np
import ml_dtypes
import concourse.bass as bass
import concourse.mybir as mybir
from concourse.bass_utils import run_bass_kernel_spmd

F32 = mybir.dt.float32
BF16 = mybir.dt.bfloat16
AF = mybir.ActivationFunctionType
ALU = mybir.AluOpType

D = 4096
KC = 32
T = 1024
TH = 512
HALO = 16
TX = T + HALO
DFF = 11008
DFE = 3584
NE = 8
NH = 32
HD = 128
S = 2048
PW = 256
EPS = 1e-6
DENSE_GROUPS = (22, 22, 21, 21)
NCORES = 8

G_POOLN, G_POOLS, G_FFN0, G_PLE0, G_KVN, G_ATTN, G_FFN1, G_PLE1 = range(8)
NG = 8


class Sem:
    __slots__ = ("h", "n", "name")

    def __init__(self, h, name):
        self.h = h
        self.n = 0
        self.name = name


class Slot:
    def __init__(self, buf, sem=None):
        self.buf = buf
        self.rel = []
        self.sem = sem
        self.marker = None


class Marker:
    __slots__ = ("fn",)

    def __init__(self):
        self.fn = None


class Ring:
    def __init__(self, slots):
        self.slots = slots
        self.i = 0

    def next(self):
        s = self.slots[self.i % len(self.slots)]
        self.i += 1
        return s


class Bld:
    ENG = ("pe", "act", "dve", "pool", "sp")

    def __init__(self):
        self.nc = bass.Bass("TRN2", target_bir_lowering=False)
        self.q = {e: [] for e in self.ENG}
        self.waited = {e: {} for e in self.ENG}
        self.sems = []
        self.sb_off = 16512
        self.sb_top = 229344
        self.nsb = 0

    def sem(self, name):
        s = Sem(self.nc.alloc_semaphore(name), name)
        self.sems.append(s)
        return s

    def sb(self, shape, dtype, off=None):
        nbytes = int(np.prod(shape[1:])) * (4 if dtype == F32 else 2)
        if off is None:
            off = self.sb_off
            self.sb_off += (nbytes + 31) // 32 * 32
            assert self.sb_off <= self.sb_top, f"SBUF overflow {self.sb_off}"
        self.nsb += 1
        return self.nc.alloc_sbuf_tensor_at(f"sb{self.nsb}", list(shape), dtype, offset=off)

    def op(self, eng, fn, waits=(), inc=None, amt=1, marker=None):
        need = {}
        for ev in waits:
            if ev is None:
                continue
            s, v = ev
            if v > need.get(s, (None, 0))[1]:
                need[s] = (s, v)
        wl = []
        wd = self.waited[eng]
        for s, v in need.values():
            if marker is not None:
                wl.append((s.h, v))
            elif wd.get(s, 0) < v:
                wd[s] = v
                wl.append((s.h, v))
        ev = None
        if inc is not None:
            inc.n += amt
            ev = (inc, inc.n)
        inch = inc.h if inc is not None else None

        def thunk(e):
            for h, v in wl:
                e.wait_ge(h, v)
            ins = fn(e)
            if inch is not None:
                ins.then_inc(inch, amt)

        if marker is not None:
            marker.fn = thunk
        else:
            self.q[eng].append(thunk)
        return ev

    def dma(self, eng, out, in_, waits, sem, accum=False, marker=None):
        if accum:
            return self.op(eng, lambda e: e.dma_start(out=out, in_=in_, accum_op=ALU.add), waits, sem, 16)
        return self.op(eng, lambda e: e.dma_start(out=out, in_=in_), waits, sem, 16, marker=marker)

    def emit(self):
        with self.nc.Block() as blk:
            for name, deco in (("pe", blk.tensor), ("act", blk.scalar), ("dve", blk.vector),
                               ("pool", blk.gpsimd), ("sp", blk.sync)):
                q = self.q[name]

                def body(e, q=q):
                    for t in q:
                        if isinstance(t, Marker):
                            if t.fn is not None:
                                t.fn(e)
                        else:
                            t(e)

                deco(body)


def evs(*xs):
    out = []
    for x in xs:
        if x is None:
            continue
        if isinstance(x, list):
            out.extend(x)
        else:
            out.append(x)
    return out


class Prog:
    def __init__(self, which, upto=99):
        self.which = which
        self.upto = upto
        b = self.b = Bld()
        nc = b.nc
        self.dram = {}
        self.s_pe = b.sem("pe")
        self.s_act = b.sem("act")
        self.s_dve = b.sem("dve")
        self.s_pool = b.sem("pool")
        self.XN = b.sb([128, KC * T], BF16)
        self.BIG2 = b.sb([128, 28 * T], BF16)
        self.big2_off = b.sb_off - 28 * T * 2
        self.WB = Ring([Slot(b.sb([128, KC * PW], BF16), b.sem(f"wld{i}")) for i in range(2)])
        self.IN = Ring([Slot(b.sb([128, T], F32), b.sem(f"inld{i}")) for i in range(3)])
        self.OUT = Ring([Slot(b.sb([128, T], F32), b.sem(f"outst{i}")) for i in range(3)])
        self.SQ = Ring([Slot(b.sb([128, T], BF16)) for i in range(2)])
        self.SG = Ring([Slot(b.sb([128, T], BF16)) for i in range(2)])
        self.RSTD = b.sb([128, T], F32)
        self.rstd_rel = []
        self.GAINS = b.sb([128, NG * KC], F32)
        self.ONES = b.sb([128, 128], BF16)
        self.SMALL = b.sb([128, 64], F32)
        self.PS = [Slot(nc.alloc_psum_tensor(f"ps{i}", [128, T], F32)) for i in range(4)]
        self.ps_i = 0
        self.s_const = b.sem("const")
        self.s_misc = b.sem("misc")
        self.h_events = []
        self.s_misc2 = b.sem("misc2")
        self.ROUT = b.sb([128, KC * NE], F32)
        self.LG = b.sb([128, 64], F32)
        self.CMB = b.sb([128, 64], F32)
        self.GT = [b.sb([128, 64], F32) for i in range(7)]
        self.IDB = b.sb([128, 128], BF16)
        self.IDF = b.sb([8, 8], F32)
        self.LT = b.sb([8, T], F32)
        self.CB = Ring([Slot(b.sb([128, T], BF16)) for i in range(2)])
        self.DG = Ring([Slot(b.sb([128, 128], BF16)) for i in range(3)])
        self.xn_rel = []
        self.xn_ready = []
        self.big2_rel = []
        self.scr_off = self.big2_off
        self.nqk = 0
        self.nv = 0
        self.perm_ap = None

    def din(self, name, shape, dtype=F32):
        t = self.b.nc.dram_tensor(name, list(shape), dtype, kind="ExternalInput")
        self.dram[name] = t
        return t.ap()

    def dout(self, name, shape, dtype=F32):
        t = self.b.nc.dram_tensor(name, list(shape), dtype, kind="ExternalOutput")
        self.dram[name] = t
        return t.ap()

    def wb_load(self, pan, kcp):
        b = self.b
        w = self.WB.next()
        ld = b.dma("pool", w.buf[:, :kcp * PW], pan, evs(w.rel), w.sem, marker=w.marker)
        w.marker = None
        return w, ld

    def wb_done(self, w, pe_ev):
        w.rel = [pe_ev]
        w.marker = Marker()
        self.b.q["pool"].append(w.marker)

    def ps_next(self, subset=(0, 1, 2, 3)):
        s = self.PS[subset[self.ps_i % len(subset)]]
        self.ps_i += 1
        return s

    def consts(self, gains_ap, small_ap):
        b = self.b
        ev1 = b.dma("sp", self.GAINS[:], gains_ap, [], self.s_const)
        ev3 = b.dma("sp", self.SMALL[:], small_ap, [], self.s_const)
        ev2 = b.op("dve", lambda e: e.memset(self.ONES[:], 1.0), [], self.s_dve)
        self.const_ev = [ev3, ev2]

    def gain(self, gi, kc):
        return self.GAINS[:, gi * KC + kc: gi * KC + kc + 1]

    def norm_phase(self, hT, gi, extra_waits=()):
        b = self.b
        dep = evs(self.h_events, list(extra_waits), self.const_ev)
        ps = self.ps_next()
        pe_ev = None
        for kc in range(KC):
            sl = self.IN.next()
            ld = b.dma("sp", sl.buf[:], hT[kc * 128:(kc + 1) * 128, :], evs(sl.rel, dep), sl.sem)
            sq = self.SQ.next()
            a = b.op("act", lambda e, i=sl.buf, o=sq.buf: e.activation(out=o[:], in_=i[:], func=AF.Square),
                     evs(ld, sq.rel), self.s_act)
            sl.rel = [a]
            for th in range(2):
                pe_ev = b.op("pe", lambda e, o=ps.buf, r=sq.buf, th=th, kc=kc: e.matmul(
                    o[:, th * TH:(th + 1) * TH], lhsT=self.ONES[:], rhs=r[:, th * TH:(th + 1) * TH],
                    start=(kc == 0), stop=(kc == KC - 1)),
                    evs(a, ps.rel if kc == 0 else None), self.s_pe)
            sq.rel = [pe_ev]
        d1 = b.op("act", lambda e, i=ps.buf: e.activation(out=self.RSTD[:], in_=i[:], func=AF.Sqrt,
                                                           bias=self.SMALL[:, 0:1], scale=1.0 / D),
                  evs(pe_ev, self.rstd_rel, self.const_ev), self.s_act)
        ps.rel = [d1]
        d2 = b.op("dve", lambda e: e.reciprocal(out=self.RSTD[:], in_=self.RSTD[:]), [d1], self.s_dve)
        last = None
        for kc in range(KC):
            sl = self.IN.next()
            ld = b.dma("sp", sl.buf[:], hT[kc * 128:(kc + 1) * 128, :], evs(sl.rel, dep), sl.sem)
            last = b.op("dve", lambda e, i=sl.buf, kc=kc: e.scalar_tensor_tensor(
                out=self.XN[:, kc * T:(kc + 1) * T], in0=i[:], scalar=self.gain(gi, kc), in1=self.RSTD[:],
                op0=ALU.mult, op1=ALU.mult), evs(ld, d2, self.xn_rel), self.s_dve)
            sl.rel = [last]
        self.rstd_rel = [last]
        self.xn_ready = [last]

    def linear(self, panels, kcp, rhs, rhs_ready, epilogue, nn_per_panel=2):
        b = self.b
        last_pe = None
        for pi, pan in enumerate(panels):
            w, ld = self.wb_load(pan, kcp)
            for nn in range(nn_per_panel):
                ps = self.ps_next()
                ev = None
                for kc in range(kcp):
                    for th in range(2):
                        first = (kc == 0 and th == 0)
                        lastmm = (kc == kcp - 1 and th == 1)
                        ev = b.op("pe", lambda e, o=ps.buf, wb=w.buf, kc=kc, nn=nn, th=th: e.matmul(
                            o[:, th * TH:(th + 1) * TH], lhsT=wb[:, kc * PW + nn * 128: kc * PW + (nn + 1) * 128],
                            rhs=rhs(kc, th), start=(kc == 0), stop=(kc == kcp - 1)),
                            evs(ld, ps.rel, rhs_ready) if first else (), self.s_pe if lastmm else None)
                last_pe = ev
                if nn == nn_per_panel - 1:
                    self.wb_done(w, last_pe)
                epilogue(pi, nn, ps, ev)
        return last_pe

    def xn_rhs(self, kc, th):
        return self.XN[:, kc * T + th * TH: kc * T + (th + 1) * TH]

    def big2_rhs(self, kc, th):
        return self.BIG2[:, kc * T + th * TH: kc * T + (th + 1) * TH]

    def accum_out(self, hT, n, ps, ev, eng):
        b = self.b
        o = self.OUT.next()
        if eng == "act":
            c = b.op("act", lambda e, i=ps.buf, ob=o.buf: e.activation(out=ob[:], in_=i[:], func=AF.Copy),
                     evs(ev, o.rel), self.s_act)
        else:
            c = b.op("dve", lambda e, i=ps.buf, ob=o.buf: e.tensor_copy(out=ob[:], in_=i[:]),
                     evs(ev, o.rel), self.s_dve)
        ps.rel = [c]
        st = b.dma("pool", hT[n * 128:(n + 1) * 128, :], o.buf[:], [c], o.sem, accum=True)
        o.rel = [st]
        self.h_events = [s.rel[0] for s in self.OUT.slots if s.rel]
        return st

    def ffn_group(self, hT, gu_panels, dn_panels, fc, comb=None):
        b = self.b
        state = {}
        act_evs = []

        def gu_epi(pi, nn, ps, ev):
            if nn == 0:
                state["g"] = (ps, ev)
                return
            psg, evg = state["g"]
            sg = self.SG.next()
            a = b.op("act", lambda e, i=psg.buf, o=sg.buf: e.activation(out=o[:], in_=i[:], func=AF.Silu),
                     evs(evg, sg.rel), self.s_act)
            psg.rel = [a]
            if comb is None:
                d = b.op("dve", lambda e, i0=sg.buf, i1=ps.buf, pi=pi: e.tensor_tensor(
                    out=self.BIG2[:, pi * T:(pi + 1) * T], in0=i0[:], in1=i1[:], op=ALU.mult),
                    evs(a, ev, self.big2_rel), self.s_dve)
                ps.rel = [d]
                sg.rel = [d]
                act_evs.append(d)
            else:
                d = b.op("dve", lambda e, i0=sg.buf, i1=ps.buf: e.tensor_tensor(
                    out=i0[:], in0=i0[:], in1=i1[:], op=ALU.mult), evs(a, ev), self.s_dve)
                ps.rel = [d]
                g = b.op("pool", lambda e, i0=sg.buf, pi=pi: e.tensor_tensor(
                    out=self.BIG2[:, pi * T:(pi + 1) * T], in0=i0[:], in1=comb[0][:], op=ALU.mult),
                    evs(d, self.big2_rel, comb[1]), self.s_pool)
                sg.rel = [g]
                act_evs.append(g)

        self.linear(gu_panels, KC, self.xn_rhs, self.xn_ready, gu_epi)
        ready = act_evs[-3:]

        def dn_epi(pi, nn, ps, ev):
            self.accum_out(hT, pi * 2 + nn, ps, ev, "act" if nn == 0 else "dve")

        last = self.linear(dn_panels, fc, self.big2_rhs, ready, dn_epi)
        self.big2_rel = [last]
        self.xn_rel = [last]


    def scratch(self, shape, dtype):
        nbytes = int(np.prod(shape[1:])) * (4 if dtype == F32 else 2)
        off = self.scr_off
        self.scr_off += (nbytes + 31) // 32 * 32
        assert self.scr_off <= self.big2_off + 28 * T * 2, "scratch overflow"
        return self.b.sb(shape, dtype, off=off)

    def scratch_reset(self):
        self.scr_off = self.big2_off

    def mixer_phase(self, xT2, tk, hT, poolw, icnt_ap):
        b = self.b
        c0 = HALO + T * tk
        xe = xT2[:, c0 - HALO: c0 + T]
        self.scratch_reset()
        XE = Ring([Slot(self.scratch([128, TX], F32), b.sem(f"xe{tk}{i}")) for i in range(2)])
        XNF = Ring([Slot(self.scratch([128, TX], F32)) for i in range(2)])
        SA = Ring([Slot(self.scratch([128, TX], F32)) for i in range(2)])
        SB = Ring([Slot(self.scratch([128, TX], F32)) for i in range(2)])
        RSX = self.scratch([128, TX], F32)
        SQX = Ring([Slot(self.scratch([128, TX], BF16)) for i in range(2)])
        ICNT = self.scratch([128, 4 * 16], F32)
        T16 = Ring([Slot(self.scratch([128, 16], F32)) for i in range(2)])
        dep = evs(self.big2_rel, self.const_ev)
        ic = b.dma("sp", ICNT[:], icnt_ap, dep, self.s_misc)
        ps = self.ps_next()
        ps2 = self.ps_next()
        pieces = [(0, HALO, ps2, 0), (HALO, HALO + TH, ps, 0), (HALO + TH, TX, ps, TH)]
        pe_ev = None
        for kc in range(KC):
            sl = XE.next()
            ld = b.dma("sp", sl.buf[:], xe[kc * 128:(kc + 1) * 128, :], evs(sl.rel, dep), sl.sem)
            sq = SQX.next()
            a = b.op("act", lambda e, i=sl.buf, o=sq.buf: e.activation(out=o[:], in_=i[:], func=AF.Square),
                     evs(ld, sq.rel), self.s_act)
            sl.rel = [a]
            for (lo, hi, pp, po) in pieces:
                pe_ev = b.op("pe", lambda e, o=pp.buf, r=sq.buf, lo=lo, hi=hi, po=po, kc=kc: e.matmul(
                    o[:, po:po + hi - lo], lhsT=self.ONES[:], rhs=r[:, lo:hi], start=(kc == 0), stop=(kc == KC - 1)),
                    evs(a, ps.rel if kc == 0 else None, ps2.rel if kc == 0 else None), self.s_pe)
            sq.rel = [pe_ev]
        d1 = b.op("act", lambda e: e.activation(out=RSX[:, HALO:TX], in_=ps.buf[:], func=AF.Sqrt,
                                                bias=self.SMALL[:, 0:1], scale=1.0 / D), evs(pe_ev), self.s_act)
        d1b = b.op("act", lambda e: e.activation(out=RSX[:, 0:HALO], in_=ps2.buf[:, 0:HALO], func=AF.Sqrt,
                                                 bias=self.SMALL[:, 0:1], scale=1.0 / D), evs(pe_ev), self.s_act)
        ps.rel = [d1]
        ps2.rel = [d1b]
        d2 = b.op("dve", lambda e: e.reciprocal(out=RSX[:], in_=RSX[:]), [d1, d1b], self.s_dve)
        last = None

        def emit_n1(kc):
            sl = XE.next()
            ld = b.dma("sp", sl.buf[:], xe[kc * 128:(kc + 1) * 128, :], evs(sl.rel, dep), sl.sem)
            xn = XNF.next()
            n1 = b.op("dve", lambda e, i=sl.buf, o=xn.buf, kc=kc: e.scalar_tensor_tensor(
                out=o[:], in0=i[:], scalar=self.gain(G_POOLN, kc), in1=RSX[:], op0=ALU.mult, op1=ALU.mult),
                evs(ld, d2, xn.rel), self.s_dve)
            sl.rel = [n1]
            return xn, n1

        pending = emit_n1(0)
        for kc in range(KC):
            g = kc // 8
            w = 2 << g
            xn, n1 = pending
            if kc + 1 < KC:
                pending = emit_n1(kc + 1)
            cur, cur_ev = xn.buf, n1
            used = []
            prev_dst = None
            for k in range(1, g + 2):
                sh = 1 << (k - 1)
                lo = (1 << k) - 1
                dst = (SA if k % 2 == 1 else SB).next()
                cur_ev = b.op("pool", lambda e, o=dst.buf, c=cur, lo=lo, sh=sh: e.tensor_tensor(
                    out=o[:, lo:TX], in0=c[:, lo:TX], in1=c[:, lo - sh:TX - sh], op=ALU.add),
                    evs(cur_ev, dst.rel), self.s_pool)
                if prev_dst is not None:
                    prev_dst.rel = [cur_ev]
                prev_dst = dst
                cur = dst.buf
            used.append(prev_dst)
            m = b.op("dve", lambda e, s_=cur, x_=xn.buf, kc=kc, w=w: e.scalar_tensor_tensor(
                out=self.XN[:, kc * T:(kc + 1) * T], in0=s_[:, HALO:TX], scalar=1.0 / w, in1=x_[:, HALO:TX],
                op0=ALU.mult, op1=ALU.subtract), evs(cur_ev, self.xn_rel), self.s_dve)
            t16 = T16.next()
            f1 = b.op("dve", lambda e, s_=cur, o=t16.buf, g=g: e.tensor_tensor(
                out=o[:], in0=s_[:, HALO:HALO + 16], in1=ICNT[:, g * 16:(g + 1) * 16], op=ALU.mult),
                evs(m, ic, t16.rel), self.s_dve)
            last = b.op("dve", lambda e, t_=t16.buf, x_=xn.buf, kc=kc: e.tensor_tensor(
                out=self.XN[:, kc * T:kc * T + 16], in0=t_[:], in1=x_[:, HALO:HALO + 16], op=ALU.subtract),
                evs(f1), self.s_dve)
            t16.rel = [last]
            xn.rel = [last]
            for u in used:
                u.rel = [last]
        ready = [last]
        lastpe = None
        for g in range(4):
            def epi(pi, nn, ps_, ev, g=g):
                n = 8 * g + 2 * pi + nn
                o = self.OUT.next()
                ld_ = b.dma("sp", o.buf[:], xe[n * 128:(n + 1) * 128, HALO:TX], evs(o.rel), o.sem)
                c = b.op("dve", lambda e, i=ps_.buf, ob=o.buf, n=n: e.scalar_tensor_tensor(
                    out=ob[:], in0=i[:], scalar=self.gain(G_POOLS, n), in1=ob[:], op0=ALU.mult, op1=ALU.add),
                    evs(ev, ld_), self.s_dve)
                ps_.rel = [c]
                st = b.dma("pool", hT[n * 128:(n + 1) * 128, :], o.buf[:], [c], o.sem)
                o.rel = [st]
            lastpe = self.linear([poolw[g, j] for j in range(4)], 8,
                                 lambda kc, th, g=g: self.XN[:, (8 * g + kc) * T + th * TH:(8 * g + kc) * T + (th + 1) * TH],
                                 ready, epi)
        self.xn_rel = [lastpe]
        self.big2_rel = evs(last, lastpe)
        self.h_events = [s.rel[0] for s in self.OUT.slots if s.rel]

    def ple_phase(self, hT, gi, pT, plew, gate_panels):
        b = self.b
        self.norm_phase(hT, gi)
        self.scratch_reset()
        PT = self.scratch([128, 2 * T], BF16)
        PWt = self.scratch([128, 2 * D], BF16)
        lds = []
        for kc in range(2):
            lds.append(b.dma("pool", PT[:, kc * T:(kc + 1) * T], pT[kc * 128:(kc + 1) * 128, :], evs(self.big2_rel), self.s_misc))
            lds.append(b.dma("pool", PWt[:, kc * D:(kc + 1) * D], plew[kc * 128:(kc + 1) * 128, :], evs(self.big2_rel), self.s_misc))
        lds = [lds[-1]]
        state = {"pe": None}

        def epi(pi, nn, ps, ev):
            n = 2 * pi + nn
            ps2 = self.ps_next()
            ev2 = None
            for kc in range(2):
                for th in range(2):
                    ev2 = b.op("pe", lambda e, o=ps2.buf, kc=kc, th=th, n=n: e.matmul(
                        o[:, th * TH:(th + 1) * TH], lhsT=PWt[:, kc * D + n * 128: kc * D + (n + 1) * 128],
                        rhs=PT[:, kc * T + th * TH: kc * T + (th + 1) * TH], start=(kc == 0), stop=(kc == 1)),
                        evs(lds, ps2.rel) if (kc == 0 and th == 0) else (),
                        self.s_pe if (kc == 1 and th == 1) else None)
            state["pe"] = ev2
            o = self.OUT.next()
            a = b.op("act", lambda e, i=ps.buf, ob=o.buf: e.activation(out=ob[:], in_=i[:], func=AF.Sigmoid),
                     evs(ev, o.rel), self.s_act)
            ps.rel = [a]
            d = b.op("dve", lambda e, i=ps2.buf, ob=o.buf: e.tensor_tensor(out=ob[:], in0=ob[:], in1=i[:], op=ALU.mult),
                     evs(a, ev2), self.s_dve)
            ps2.rel = [d]
            st = b.dma("pool", hT[n * 128:(n + 1) * 128, :], o.buf[:], [d], o.sem, accum=True)
            o.rel = [st]

        last = self.linear(gate_panels, KC, self.xn_rhs, self.xn_ready, epi)
        self.xn_rel = [last]
        self.big2_rel = evs(last, state["pe"])
        self.h_events = [s.rel[0] for s in self.OUT.slots if s.rel]

    def qk_phase(self, panels, gcol, rope_ap, dstT, col0):
        b = self.b
        self.scratch_reset()
        COS = self.scratch([128, T], F32)
        SIN = self.scratch([128, T], F32)
        PERM = self.scratch([128, 128], BF16)
        RS = [Slot(self.scratch([128, T], F32)) for i in range(2)]
        KN = [Slot(self.scratch([128, T], F32)) for i in range(2)]
        KNB = [Slot(self.scratch([128, T], BF16)) for i in range(2)]
        T1 = [Slot(self.scratch([128, T], F32)) for i in range(2)]
        T2 = [Slot(self.scratch([128, T], F32)) for i in range(2)]
        KO = [Slot(self.scratch([128, T], BF16), b.sem(f"ko{self.nqk}{i}")) for i in range(2)]
        self.nqk += 1
        dep = evs(self.big2_rel)
        l1 = b.dma("sp", COS[:], rope_ap[0], dep, self.s_misc)
        l2 = b.dma("sp", SIN[:], rope_ap[1], dep, self.s_misc)
        l3 = b.dma("pool", PERM[:], self.perm_ap, dep, self.s_misc2)
        tabs = [l2, l3]
        psK = [self.PS[0], self.PS[1]]
        psS, psR = self.PS[2], self.PS[3]
        st = {}
        wslot = None
        ld = None
        lastpe = None
        last_evs = []
        SPLIT = 6
        for i in range(NH + 2):
            ps = None
            if i < NH:
                pi, nn = i // 2, i % 2
                if nn == 0:
                    wslot, ld = self.wb_load(panels[pi], KC)
                ps = psK[i % 2]

            def emit_main(lo, hi, ps=ps, i=i):
                ev = None
                nn = i % 2
                for kc in range(lo, hi):
                    for th in range(2):
                        first = (kc == 0 and th == 0)
                        lastmm = (kc == KC - 1 and th == 1)
                        ev = b.op("pe", lambda e, o=ps.buf, wb=wslot.buf, kc=kc, nn=nn, th=th: e.matmul(
                            o[:, th * TH:(th + 1) * TH], lhsT=wb[:, kc * PW + nn * 128: kc * PW + (nn + 1) * 128],
                            rhs=self.xn_rhs(kc, th), start=(kc == 0), stop=(kc == KC - 1)),
                            evs(ld, ps.rel, self.xn_ready) if first else (), self.s_pe if lastmm else None)
                return ev

            if i < NH:
                emit_main(0, SPLIT)
            if i >= 2:
                j = i - 2
                s_ = st[j]
                p2 = None
                for th in range(2):
                    p2 = b.op("pe", lambda e, r=s_["knb"].buf, th=th: e.matmul(
                        psR.buf[:, th * TH:(th + 1) * TH], lhsT=PERM[:], rhs=r[:, th * TH:(th + 1) * TH],
                        start=True, stop=True), evs(s_["a3"], psR.rel, tabs) if th == 0 else (),
                        self.s_pe if th == 1 else None)
                s_["knb"].rel = [p2]
                t2, ko = T2[j % 2], KO[j % 2]
                d3 = b.op("dve", lambda e, o=t2.buf: e.tensor_tensor(out=o[:], in0=psR.buf[:], in1=SIN[:], op=ALU.mult),
                          evs(p2, t2.rel, tabs), self.s_dve)
                psR.rel = [d3]
                g2 = b.op("pool", lambda e, a_=s_["t1"].buf, b_=t2.buf, o=ko.buf: e.tensor_tensor(
                    out=o[:], in0=a_[:], in1=b_[:], op=ALU.add), evs(s_["g1"], d3, ko.rel), self.s_pool)
                s_["t1"].rel = [g2]
                t2.rel = [g2]
                stv = b.dma("sp", dstT[j, :, col0:col0 + T], ko.buf[:], [g2], ko.sem)
                ko.rel = [stv]
                last_evs = [k.rel[0] for k in KO if k.rel]
                del st[j]
            if 1 <= i <= NH:
                j = i - 1
                s_ = st[j]
                p1 = None
                for th in range(2):
                    p1 = b.op("pe", lambda e, r=s_["sq"].buf, th=th: e.matmul(
                        psS.buf[:, th * TH:(th + 1) * TH], lhsT=self.ONES[:], rhs=r[:, th * TH:(th + 1) * TH],
                        start=True, stop=True), evs(s_["a1"], psS.rel) if th == 0 else (), self.s_pe if th == 1 else None)
                s_["sq"].rel = [p1]
                rs, kn, knb = RS[j % 2], KN[j % 2], KNB[j % 2]
                a2 = b.op("act", lambda e, o=rs.buf: e.activation(out=o[:], in_=psS.buf[:], func=AF.Sqrt,
                                                                  bias=self.SMALL[:, 0:1], scale=1.0 / HD),
                          evs(p1, rs.rel), self.s_act)
                psS.rel = [a2]
                d1 = b.op("dve", lambda e, o=rs.buf: e.reciprocal(out=o[:], in_=o[:]), [a2], self.s_dve)
                d2 = b.op("dve", lambda e, i_=s_["ps"].buf, o=kn.buf, r=rs.buf: e.scalar_tensor_tensor(
                    out=o[:], in0=i_[:], scalar=self.SMALL[:, gcol:gcol + 1], in1=r[:], op0=ALU.mult, op1=ALU.mult),
                    evs(d1, s_["ev"], kn.rel, self.const_ev), self.s_dve)
                s_["ps"].rel = [d2]
                rs.rel = [d2]
                a3 = b.op("act", lambda e, i_=kn.buf, o=knb.buf: e.activation(out=o[:], in_=i_[:], func=AF.Copy),
                          evs(d2, knb.rel), self.s_act)
                t1 = T1[j % 2]
                g1 = b.op("pool", lambda e, i_=kn.buf, o=t1.buf: e.tensor_tensor(out=o[:], in0=i_[:], in1=COS[:], op=ALU.mult),
                          evs(d2, tabs, t1.rel), self.s_pool)
                kn.rel = [a3, g1]
                s_.update(a3=a3, g1=g1, knb=knb, t1=t1)
            if i < NH:
                ev = emit_main(SPLIT, KC)
                lastpe = ev
                if nn == 1:
                    self.wb_done(wslot, ev)
                sq = self.SQ.next()
                a1 = b.op("act", lambda e, i_=ps.buf, o=sq.buf: e.activation(out=o[:], in_=i_[:], func=AF.Square),
                          evs(ev, sq.rel), self.s_act)
                st[i] = dict(ps=ps, ev=ev, sq=sq, a1=a1)
        self.xn_rel = [lastpe]
        self.big2_rel = evs(last_evs, lastpe)
        return last_evs

    def v_phase(self, panels, Vd, row0):
        b = self.b
        VST = Ring([Slot(self.scratch([128, T], BF16), b.sem(f"vst{self.nv}{i}")) for i in range(2)])
        self.nv += 1
        lastpe = None
        cnt = 0
        for pi, pan in enumerate(panels):
            w, ld = self.wb_load(pan, KC)
            for hh in range(2):
                ps = self.ps_next()
                ev = None
                for tt in range(4):
                    tok = (4 * hh + tt) * 128
                    for kc in range(KC):
                        first = (tt == 0 and kc == 0)
                        lastmm = (tt == 3 and kc == KC - 1)
                        ev = b.op("pe", lambda e, o=ps.buf, wb=w.buf, kc=kc, tt=tt, tok=tok: e.matmul(
                            o[:, tt * PW:(tt + 1) * PW], lhsT=self.XN[:, kc * T + tok: kc * T + tok + 128],
                            rhs=wb[:, kc * PW:(kc + 1) * PW], start=(kc == 0), stop=(kc == KC - 1)),
                            evs(ld, ps.rel, self.xn_ready) if first else (), self.s_pe if lastmm else None)
                lastpe = ev
                if hh == 1:
                    self.wb_done(w, lastpe)
                v = VST.next()
                if cnt % 2 == 0:
                    c = b.op("act", lambda e, i=ps.buf, o=v.buf: e.activation(out=o[:], in_=i[:], func=AF.Copy),
                             evs(ev, v.rel), self.s_act)
                else:
                    c = b.op("dve", lambda e, i=ps.buf, o=v.buf: e.tensor_copy(out=o[:], in_=i[:]),
                             evs(ev, v.rel), self.s_dve)
                cnt += 1
                ps.rel = [c]
                r0 = row0 + 4 * hh * 128
                dst = Vd[r0:r0 + 512, pi * PW:(pi + 1) * PW].rearrange("(tt p) c -> p tt c", p=128)
                src = v.buf[:, :].rearrange("p (tt c) -> p tt c", c=PW)
                stv = b.dma("sp", dst, src, [c], v.sem)
                v.rel = [stv]
        self.xn_rel = [lastpe]
        outs = [s.rel[0] for s in VST.slots if s.rel]
        self.big2_rel = evs(self.big2_rel, outs, lastpe)
        return outs

    def attn_phase(self, KT, Vd, QT, mask_ap, valid_ap, kv_ready):
        b = self.b
        self.scratch_reset()
        MSK = self.scratch([128, 19 * 128], BF16)
        VAL = self.scratch([128, 16], F32)
        KTh = Ring([Slot(self.scratch([128, S], BF16), b.sem(f"kth{i}")) for i in range(2)])
        Vh = Ring([Slot(self.scratch([128, 16 * 128], BF16), b.sem(f"vh{i}")) for i in range(2)])
        Qh = Ring([Slot(self.scratch([128, T], BF16), b.sem(f"qh{i}")) for i in range(2)])
        EB = Ring([Slot(self.scratch([128, TH], BF16)) for i in range(3)])
        PB = Ring([Slot(self.scratch([128, TH], BF16)) for i in range(3)])
        RD = Ring([Slot(self.scratch([128, TH], F32)) for i in range(2)])
        dep = evs(self.big2_rel, kv_ready)
        m1 = b.dma("pool", MSK[:], mask_ap, dep, self.s_misc2)
        m2 = b.dma("sp", VAL[:], valid_ap, dep, self.s_misc)
        psND = [self.PS[0], self.PS[1]]
        psS = [(self.PS[2], 0), (self.PS[2], 1), (self.PS[3], 0), (self.PS[3], 1)]
        srel = [list(self.PS[2].rel), list(self.PS[2].rel), list(self.PS[3].rel), list(self.PS[3].rel)]
        scale = 1.0 / float(np.sqrt(HD))
        si = 0
        nd_i = 0
        cnt = 0
        last = None
        lastpe = None
        for h in range(NH):
            k_, v_, q_ = KTh.next(), Vh.next(), Qh.next()
            lk = b.dma("sp", k_.buf[:], KT[h], evs(k_.rel, dep), k_.sem)
            lv = b.dma("sp", v_.buf[:, :].rearrange("p (j d) -> p j d", d=128),
                       Vd[:, h * 128:(h + 1) * 128].rearrange("(j p) d -> p j d", p=128), evs(v_.rel, dep), v_.sem)
            lq = b.dma("sp", q_.buf[:], QT[h], evs(q_.rel, dep), q_.sem)
            for c in range(2):
                nkb = 12 + 4 * c
                nd = psND[nd_i % 2]
                nd_i += 1
                sevs = {}

                def emit_s(jj):
                    nonlocal si
                    pslot, half = psS[si % 4]
                    idx = si % 4
                    si += 1
                    ev = b.op("pe", lambda e, o=pslot.buf, half=half, jj=jj, kb=k_.buf, qb=q_.buf, c=c: e.matmul(
                        o[:, half * TH:(half + 1) * TH], lhsT=kb[:, jj * 128:(jj + 1) * 128],
                        rhs=qb[:, c * TH:(c + 1) * TH], start=True, stop=True),
                        evs(lk, lq, srel[idx]), self.s_pe)
                    sevs[jj] = (pslot, half, idx, ev)

                emit_s(0)
                emit_s(1)
                for jj in range(nkb):
                    if jj + 2 < nkb:
                        emit_s(jj + 2)
                    pslot, half, idx, sev = sevs[jj]
                    eb = EB.next()
                    a = b.op("act", lambda e, i=pslot.buf, half=half, o=eb.buf: e.activation(
                        out=o[:], in_=i[:, half * TH:(half + 1) * TH], func=AF.Exp, scale=scale),
                        evs(sev, eb.rel), self.s_act)
                    srel[idx] = [a]
                    pb = PB.next()
                    dd = 8 + 4 * c - jj
                    if jj >= 8 and jj % 2 == 1:
                        d = b.op("pool", lambda e, i=eb.buf, o=pb.buf, dd=dd: e.tensor_tensor(
                            out=o[:], in0=i[:], in1=MSK[:, (dd + 3) * 128:(dd + 3) * 128 + TH], op=ALU.mult),
                            evs(a, pb.rel, m1), self.s_pool)
                    else:
                        d = b.op("dve", lambda e, i=eb.buf, o=pb.buf, jj=jj, dd=dd: e.scalar_tensor_tensor(
                            out=o[:], in0=i[:], scalar=VAL[:, jj:jj + 1], in1=MSK[:, (dd + 3) * 128:(dd + 3) * 128 + TH],
                            op0=ALU.mult, op1=ALU.mult), evs(a, pb.rel, m1, m2), self.s_dve)
                    eb.rel = [d]
                    b.op("pe", lambda e, o=nd.buf, jj=jj, p=pb.buf, vb=v_.buf, nkb=nkb: e.matmul(
                        o[:, 0:TH], lhsT=vb[:, jj * 128:(jj + 1) * 128], rhs=p[:], start=(jj == 0), stop=(jj == nkb - 1)),
                        evs(d, lv, nd.rel if jj == 0 else None), None)
                    lastpe = b.op("pe", lambda e, o=nd.buf, jj=jj, p=pb.buf, nkb=nkb: e.matmul(
                        o[:, TH:2 * TH], lhsT=self.ONES[:], rhs=p[:], start=(jj == 0), stop=(jj == nkb - 1)),
                        (), self.s_pe)
                    pb.rel = [lastpe]
                rd = RD.next()
                r1 = b.op("dve", lambda e, i=nd.buf, o=rd.buf: e.reciprocal(out=o[:], in_=i[:, TH:2 * TH]),
                          evs(lastpe, rd.rel), self.s_dve)
                last = b.op("dve", lambda e, i=nd.buf, r=rd.buf, h=h, c=c: e.tensor_tensor(
                    out=self.XN[:, h * T + c * TH: h * T + (c + 1) * TH], in0=i[:, 0:TH], in1=r[:], op=ALU.mult),
                    evs(r1, self.xn_rel), self.s_dve)
                rd.rel = [last]
                nd.rel = [last]
            k_.rel = [lastpe]
            v_.rel = [lastpe]
            q_.rel = [lastpe]
        for idx in range(4):
            psS[idx][0].rel = evs(psS[idx][0].rel, srel[idx])
        self.xn_ready = [last]
        self.big2_rel = evs(lastpe, last)

    def simple_linear_accum(self, hT, panels):
        def epi(pi, nn, ps, ev):
            self.accum_out(hT, pi * 2 + nn, ps, ev, "act" if nn == 0 else "dve")
        last = self.linear(panels, KC, self.xn_rhs, self.xn_ready, epi)
        self.xn_rel = [last]

    def moe_phase(self, hT, gi, router_ap, ident_ap, moe_gu, moe_dn):
        b = self.b
        self.scratch_reset()
        ROUT, LG, IDB, CMB = self.ROUT, self.LG, self.IDB, self.CMB
        lr = b.dma("sp", ROUT[:], router_ap, evs(self.const_ev), self.s_misc)
        li = b.dma("pool", IDB[:], ident_ap, [], self.s_misc2)
        dep = evs(self.h_events, self.const_ev)
        ps = self.ps_next()
        pe_ev = None
        for kc in range(KC):
            sl = self.IN.next()
            ld = b.dma("sp", sl.buf[:], hT[kc * 128:(kc + 1) * 128, :], evs(sl.rel, dep), sl.sem)
            sq = self.SQ.next()
            a = b.op("act", lambda e, i=sl.buf, o=sq.buf: e.activation(out=o[:], in_=i[:], func=AF.Square),
                     evs(ld, sq.rel), self.s_act)
            sl.rel = [a]
            for th in range(2):
                pe_ev = b.op("pe", lambda e, o=ps.buf, r=sq.buf, th=th, kc=kc: e.matmul(
                    o[:, th * TH:(th + 1) * TH], lhsT=self.ONES[:], rhs=r[:, th * TH:(th + 1) * TH],
                    start=(kc == 0), stop=(kc == KC - 1)), evs(a, ps.rel if kc == 0 else None), self.s_pe)
            sq.rel = [pe_ev]
        d1 = b.op("act", lambda e, i=ps.buf: e.activation(out=self.RSTD[:], in_=i[:], func=AF.Sqrt,
                                                           bias=self.SMALL[:, 0:1], scale=1.0 / D),
                  evs(pe_ev, self.rstd_rel), self.s_act)
        ps.rel = [d1]
        d2 = b.op("dve", lambda e: e.reciprocal(out=self.RSTD[:], in_=self.RSTD[:]), [d1], self.s_dve)
        psl = self.ps_next()
        last = None
        lpe = None
        for kc in range(KC):
            sl = self.IN.next()
            ld = b.dma("sp", sl.buf[:], hT[kc * 128:(kc + 1) * 128, :], evs(sl.rel, dep), sl.sem)
            n1 = b.op("dve", lambda e, i=sl.buf, kc=kc: e.scalar_tensor_tensor(
                out=i[:], in0=i[:], scalar=self.gain(gi, kc), in1=self.RSTD[:], op0=ALU.mult, op1=ALU.mult),
                evs(ld, d2), self.s_dve)
            last = b.op("act", lambda e, i=sl.buf, kc=kc: e.activation(out=self.XN[:, kc * T:(kc + 1) * T], in_=i[:], func=AF.Copy),
                        evs(n1, self.xn_rel), self.s_act)
            for th in range(2):
                lpe = b.op("pe", lambda e, i=sl.buf, th=th, kc=kc: e.matmul(
                    psl.buf[0:8, th * TH:(th + 1) * TH], lhsT=ROUT[:, kc * 8:(kc + 1) * 8], rhs=i[:, th * TH:(th + 1) * TH],
                    start=(kc == 0), stop=(kc == KC - 1)),
                    evs(n1, lr, psl.rel if kc == 0 else None) if th == 0 else (), self.s_pe if th == 1 else None)
            sl.rel = [last, lpe]
        self.rstd_rel = [n1]
        self.xn_ready = [last]
        lt = b.op("dve", lambda e: e.tensor_copy(out=self.LT[:], in_=psl.buf[0:8, :]), [lpe], self.s_dve)
        psl.rel = [lt]
        lif = b.dma("sp", self.IDF[:], ident_ap[0:8, 0:8], [], self.s_misc)
        psl2 = self.ps_next()
        tp = None
        for tt in range(8):
            tp = b.op("pe", lambda e, tt=tt: e.matmul(
                psl2.buf[:, tt * 8:(tt + 1) * 8], lhsT=self.LT[0:8, tt * 128:(tt + 1) * 128], rhs=self.IDF[0:8, 0:8],
                start=True, stop=True), evs(lt, lif, psl2.rel) if tt == 0 else (), self.s_pe if tt == 7 else None)
        M1, M2, EQ, L2, NEG, EX, DEN = self.GT
        v3 = lambda t: t[:, 0:64].rearrange("p (a e) -> p a e", e=8)
        bc = lambda t: t[:, 0:8].unsqueeze(2).to_broadcast([128, 8, 8])
        c0 = b.op("dve", lambda e: e.tensor_copy(out=LG[:, 0:64], in_=psl2.buf[:, 0:64]), [tp], self.s_dve)
        psl2.rel = [c0]
        c1 = b.op("dve", lambda e: e.tensor_reduce(out=M1[:, 0:8], in_=v3(LG), axis=mybir.AxisListType.X, op=ALU.max), [c0], self.s_dve)
        c2 = b.op("dve", lambda e: e.tensor_tensor(out=v3(EQ), in0=v3(LG), in1=bc(M1), op=ALU.is_equal), [c1], self.s_dve)
        c3 = b.op("dve", lambda e: e.scalar_tensor_tensor(out=L2[:, 0:64], in0=EQ[:, 0:64], scalar=-1e30, in1=LG[:, 0:64],
                                                           op0=ALU.mult, op1=ALU.add), [c2], self.s_dve)
        c4 = b.op("dve", lambda e: e.tensor_reduce(out=M2[:, 0:8], in_=v3(L2), axis=mybir.AxisListType.X, op=ALU.max), [c3], self.s_dve)
        c5 = b.op("dve", lambda e: e.tensor_tensor(out=v3(EQ), in0=v3(LG), in1=bc(M2), op=ALU.is_ge), [c4], self.s_dve)
        c6 = b.op("dve", lambda e: e.tensor_tensor(out=v3(L2), in0=v3(LG), in1=bc(M1), op=ALU.subtract), [c5], self.s_dve)
        c7 = b.op("act", lambda e: e.activation(out=EX[:, 0:64], in_=L2[:, 0:64], func=AF.Exp), [c6], self.s_act)
        c8 = b.op("dve", lambda e: e.tensor_tensor(out=EX[:, 0:64], in0=EX[:, 0:64], in1=EQ[:, 0:64], op=ALU.mult), [c7], self.s_dve)
        c9 = b.op("dve", lambda e: e.tensor_reduce(out=DEN[:, 0:8], in_=v3(EX), axis=mybir.AxisListType.X, op=ALU.add), [c8], self.s_dve)
        c10 = b.op("dve", lambda e: e.reciprocal(out=DEN[:, 0:8], in_=DEN[:, 0:8]), [c9], self.s_dve)
        c11 = b.op("dve", lambda e: e.tensor_tensor(out=v3(CMB), in0=v3(EX), in1=bc(DEN), op=ALU.mult), [c10], self.s_dve)
        DG = self.DG
        for ex in range(NE):
            cb = self.CB.next()
            psc = self.ps_next()
            pe = None
            for tt in range(8):
                dg = DG.next()
                dv = b.op("dve", lambda e, o=dg.buf, tt=tt, ex=ex: e.tensor_scalar_mul(
                    out=o[:], in0=IDB[:], scalar1=CMB[:, tt * 8 + ex: tt * 8 + ex + 1]),
                    evs(c11, li, dg.rel), self.s_dve)
                pe = b.op("pe", lambda e, o=psc.buf, r=dg.buf, tt=tt: e.matmul(
                    o[:, tt * 128:(tt + 1) * 128], lhsT=self.ONES[:], rhs=r[:], start=True, stop=True),
                    evs(dv, psc.rel if tt == 0 else None), self.s_pe)
                dg.rel = [pe]
            cc = b.op("act", lambda e, i=psc.buf, o=cb.buf: e.activation(out=o[:], in_=i[:], func=AF.Copy),
                      evs(pe, cb.rel), self.s_act)
            psc.rel = [cc]
            self.ffn_group(hT, [moe_gu[ex, f] for f in range(28)], [moe_dn[ex, j] for j in range(D // PW)], 28,
                           comb=(cb.buf, cc))
            cb.rel = list(self.big2_rel)

    def finish(self, final_events):
        b = self.b
        b.op("sp", lambda e: e.nop(), evs(final_events), None)
        b.emit()
        return b.nc


def tile_w(w, pw=PW):
    K, N = w.shape
    kc = K // 128
    npan = N // pw
    return np.ascontiguousarray(w.reshape(kc, 128, npan, pw).transpose(2, 1, 0, 3)).reshape(npan, 128, kc * pw)


def tile_gu(w, dff):
    K = w.shape[0]
    g = w[:, :dff].reshape(K, dff // 128, 1, 128)
    u = w[:, dff:].reshape(K, dff // 128, 1, 128)
    return tile_w(np.concatenate([g, u], axis=2).reshape(K, 2 * dff))


def fm(v):
    return np.ascontiguousarray(v.reshape(-1, 128).T)


def build_full(upto=99, tiles=(0, 1), debug=False):
    P = Prog("F", upto)
    b = P.b
    nc = b.nc
    NP16 = D // PW
    xT2 = P.din("xT2", [D, HALO + 2 * T])
    gains = P.din("gains", [128, NG * KC])
    small = P.din("small", [128, 64])
    icnt = P.din("icnt", [2, 128, 64])
    poolw = P.din("poolw", [4, 4, 128, 8 * PW])
    hT = P.dout("hT", [D, T])
    hprev = nc.dram_tensor("hprev", [D, T], F32).ap()
    if debug:
        KT = P.dout("KTs", [NH, 128, S], BF16)
        Vd = P.dout("Vds", [S, D], BF16)
        QT = P.dout("QTs", [NH, 128, T], BF16)
    else:
        KT = nc.dram_tensor("KTs", [NH, 128, S], BF16).ap()
        Vd = nc.dram_tensor("Vds", [S, D], BF16).ap()
        QT = nc.dram_tensor("QTs", [NH, 128, T], BF16).ap()
    P.consts(gains, small)
    kv_events = []
    stage = 0

    def more():
        nonlocal stage
        stage += 1
        return stage <= upto

    done = False
    for tk in tiles:
        h = hprev if tk == 0 else hT
        if not more():
            done = True
            break
        P.mixer_phase(xT2, tk, h, poolw, icnt[tk])
        if not more():
            done = True
            break
        if tk == tiles[0]:
            wgu0 = P.din("wgu0", [DFF // 128, 128, KC * PW])
            wdn0 = [P.din(f"wdn0_{g}", [NP16, 128, fc * PW]) for g, fc in enumerate(DENSE_GROUPS)]
        P.norm_phase(h, G_FFN0)
        f0 = 0
        for g, fc in enumerate(DENSE_GROUPS):
            P.ffn_group(h, [wgu0[f0 + i] for i in range(fc)], [wdn0[g][j] for j in range(NP16)], fc)
            f0 += fc
        if not more():
            done = True
            break
        if tk == tiles[0]:
            p0T = P.din("p0T", [256, 2 * T])
            plew0 = P.din("plew0", [256, D])
            pleg0 = P.din("pleg0", [NP16, 128, KC * PW])
        P.ple_phase(h, G_PLE0, p0T[:, tk * T:(tk + 1) * T], plew0, [pleg0[j] for j in range(NP16)])
        if not more():
            done = True
            break
        if tk == tiles[0]:
            wk = P.din("wk", [NP16, 128, KC * PW])
            wv = P.din("wv", [NP16, 128, KC * PW])
            rope = P.din("rope", [2, 2, 128, T])
            P.perm_ap = P.din("perm", [128, 128])
        P.norm_phase(h, G_KVN)
        kst = P.qk_phase([wk[j] for j in range(NP16)], 1, rope[tk], KT, tk * T)
        vst = P.v_phase([wv[j] for j in range(NP16)], Vd, tk * T)
        kv_events += kst + vst
    fin = None
    while not done:
        if not more():
            break
        wq = P.din("wq", [NP16, 128, KC * PW])
        mask = P.din("mask", [128, 19 * 128])
        valid = P.din("valid", [128, 16])
        P.norm_phase(hT, G_ATTN)
        qst = P.qk_phase([wq[j] for j in range(NP16)], 2, rope[1], QT, 0)
        P.attn_phase(KT, Vd, QT, mask, valid, kv_events + qst)
        if not more():
            break
        wo = P.din("wo", [NP16, 128, KC * PW])
        P.simple_linear_accum(hT, [wo[j] for j in range(NP16)])
        if not more():
            break
        router = P.din("router", [128, KC * NE])
        ident = P.din("ident", [128, 128])
        moegu = P.din("moegu", [NE, DFE // 128, 128, KC * PW])
        moedn = P.din("moedn", [NE, NP16, 128, (DFE // 128) * PW])
        P.moe_phase(hT, G_FFN1, router, ident, moegu, moedn)
        if not more():
            break
        p1T = P.din("p1T", [256, T])
        plew1 = P.din("plew1", [256, D])
        pleg1 = P.din("pleg1", [NP16, 128, KC * PW])
        P.ple_phase(hT, G_PLE1, p1T, plew1, [pleg1[j] for j in range(NP16)])
        break
    if upto == 9:
        pass
    nc_ = P.finish(evs(P.h_events, kv_events, list(P.xn_ready)))
    return nc_, sorted(P.dram.keys())


def _mult(delta):
    m = np.zeros_like(delta, dtype=np.float32)
    m += ((delta >= 0) & (delta <= 128)).astype(np.float32)
    m += ((delta >= 0) & (delta % 4 == 0) & (delta <= 512)).astype(np.float32)
    m += ((delta >= 0) & (delta % 16 == 0) & (delta <= 2048)).astype(np.float32)
    return m


def host_inputs(inp, names, cores):
    x = inp["x"]
    p = inp["p"]
    shared = {}
    g = np.zeros((128, NG * KC), np.float32)
    for gi, v in ((G_POOLN, inp["pool_norm"][0]), (G_POOLS, inp["pool_scale"][0]), (G_FFN0, inp["ffn_norm"][0]),
                  (G_PLE0, inp["ple_norm"][0]), (G_KVN, inp["kv_norm"]), (G_ATTN, inp["attn_norm"][0]),
                  (G_FFN1, inp["ffn_norm"][1]), (G_PLE1, inp["ple_norm"][1])):
        g[:, gi * KC:(gi + 1) * KC] = fm(np.asarray(v, np.float32))
    shared["gains"] = g
    sm = np.zeros((128, 64), np.float32)
    sm[:, 0] = EPS
    sm[:, 1] = inp["k_norm"]
    sm[:, 2] = inp["q_norm"][0]
    shared["small"] = sm
    if "poolw" in names:
        shared["poolw"] = np.stack([tile_w(inp["pool_w"][0, gg]) for gg in range(4)])
    if "wgu0" in names:
        shared["wgu0"] = tile_gu(inp["dense_w_gate_up"][0], DFF)
        f0 = 0
        for gg, fc in enumerate(DENSE_GROUPS):
            shared[f"wdn0_{gg}"] = tile_w(inp["dense_w_down"][0][f0 * 128:(f0 + fc) * 128, :])
            f0 += fc
    for l in (0, 1):
        if f"plew{l}" in names:
            shared[f"plew{l}"] = np.ascontiguousarray(inp["ple_w"][l])
            shared[f"pleg{l}"] = tile_w(inp["ple_gate_w"][l])
    if "wk" in names:
        shared["wk"] = tile_w(inp["w_kv"][:, :D])
        shared["wv"] = tile_w(inp["w_kv"][:, D:])
        pm = np.zeros((128, 128), np.float32)
        for m in range(128):
            pm[(m + 64) % 128, m] = 1.0
        shared["perm"] = pm
    if "wq" in names:
        shared["wq"] = tile_w(inp["w_q"][0])
        kj = np.arange(128)[:, None]
        qi = np.arange(128)[None, :]
        strip = np.concatenate([_mult(128 * d + qi - kj) for d in range(-3, 16)], axis=1)
        shared["mask"] = np.ascontiguousarray(strip.astype(np.float32))
    if "wo" in names:
        shared["wo"] = tile_w(inp["w_o"][0])
    if "router" in names:
        r = inp["moe_router"][0]
        shared["router"] = np.ascontiguousarray(r.reshape(KC, 128, NE).transpose(1, 0, 2)).reshape(128, KC * NE)
        shared["ident"] = np.eye(128, dtype=np.float32)
        shared["moegu"] = np.stack([tile_gu(inp["moe_w_gate_up"][0, e], DFE) for e in range(NE)])
        shared["moedn"] = np.stack([tile_w(inp["moe_w_down"][0, e]) for e in range(NE)])
    half = HD // 2
    inv_freq = np.exp(-np.log(np.float32(10000.0)) * np.arange(half, dtype=np.float32) / np.float32(half)).astype(np.float32)
    in_maps = []
    for c in cores:
        bb, hf = c // 2, c % 2
        m = dict(shared)
        xt = np.zeros((D, HALO + 2 * T), np.float32)
        if hf == 1:
            xt[:, HALO:HALO + T] = x[bb, 0:T, :].T
        xt[:, HALO + T:] = x[bb, hf * T:(hf + 1) * T, :].T
        m["xT2"] = xt
        ic = np.zeros((2, 128, 64), np.float32)
        for gg in range(4):
            w = 2 << gg
            start = 1.0 / np.minimum(np.arange(16) + 1, w).astype(np.float32)
            ic[0, :, gg * 16:(gg + 1) * 16] = start
            ic[1, :, gg * 16:(gg + 1) * 16] = start if hf == 0 else np.float32(1.0 / w)
        m["icnt"] = ic
        if "p0T" in names:
            pt = np.zeros((256, 2 * T), np.float32)
            if hf == 1:
                pt[:, :T] = p[0, bb, 0:T, :].T
            pt[:, T:] = p[0, bb, hf * T:(hf + 1) * T, :].T
            m["p0T"] = pt
        if "p1T" in names:
            m["p1T"] = np.ascontiguousarray(p[1, bb, hf * T:(hf + 1) * T, :].T)
        if "rope" in names:
            rp = np.zeros((2, 2, 128, T), np.float32)
            for tk in range(2):
                pos0 = 0 if tk == 0 else hf * T
                ang = (np.arange(pos0, pos0 + T, dtype=np.float32)[:, None] * inv_freq[None, :]).astype(np.float32)
                cs = np.cos(ang).T.astype(np.float32)
                sn = np.sin(ang).T.astype(np.float32)
                rp[tk, 0] = np.concatenate([cs, cs], axis=0)
                rp[tk, 1] = np.concatenate([-sn, sn], axis=0)
            m["rope"] = rp
        if "valid" in names:
            v = np.ones((128, 16), np.float32)
            if hf == 0:
                v[:, :8] = 0.0
            m["valid"] = v
        in_maps.append({k: m[k] for k in names if k not in ("hT", "KTs", "Vds", "QTs")})
    return in_maps


_CACHE = {}


def kernel(**inputs):
    inp = {k: np.asarray(v) for k, v in inputs.items()}
    if "prog" not in _CACHE:
        _CACHE["prog"] = build_full()
    nc, names = _CACHE["prog"]
    cores = list(range(NCORES))
    in_maps = host_inputs(inp, names, cores)
    res = run_bass_kernel_spmd(nc, in_maps, core_ids=cores)
    out = np.empty((4, S, D), np.float32)
    for c in cores:
        bb, hf = c // 2, c % 2
        out[bb, hf * T:(hf + 1) * T, :] = res.results[c]["hT"].T
    return out


def build_test_ffn():
    P = Prog("T")
    b = P.b
    xT = P.din("xT", [D, T])
    gains = P.din("gains", [128, NG * KC])
    wgu = P.din("wgu", [DFF // 128, 128, KC * PW])
    wdn = [P.din(f"wdn{g}", [D // PW, 128, fc * PW]) for g, fc in enumerate(DENSE_GROUPS)]
    hT = P.dout("hT", [D, T])
    P.consts(gains)
    P.xn_rel = []
    P.big2_rel = []
    c = b.dma("sp", hT[:, :], xT[:, :], [], P.s_misc)
    P.h_events = [c]
    P.norm_phase(hT, G_FFN0)
    f0 = 0
    for g, fc in enumerate(DENSE_GROUPS):
        if g >= P.upto:
            break
        P.ffn_group(hT, [wgu[f0 + i] for i in range(fc)], [wdn[g][j] for j in range(D // PW)], fc)
        f0 += fc
    return P.finish(P.h_events)
```
